# Optimizing a Trainium2 kernel written in Bass

```python
import jax
import jax.numpy as jnp
from jax import lax
import numpy as np

D_MODEL = 1024
BATCH = 8
SEQ = 2048
DEPTH = 4

GRID_W = 64
CTX_LEN = 256
Q_BLOCK = 128
NORM_EPS = 1e-6
NEG_INF = -1e30

A_HEAD_DIM = 64
A_DIM = D_MODEL // 2
A_HEADS = A_DIM // A_HEAD_DIM
LORA_W = 64
LORA_A = 64
LORA_G = 128
GN_EPS = 64e-5

QK_NOPE = 64
QK_ROPE = 32
V_HEAD = 64
B_DIM = D_MODEL - A_DIM
B_HEADS = B_DIM // V_HEAD
Q_RANK = 384
KV_RANK = 256
ROPE_BASE = 10000.0

A_COLS = 3 * A_DIM + 2 * LORA_W + 2 * LORA_A + LORA_G
AB_COLS = A_COLS + Q_RANK + KV_RANK + QK_ROPE
MIX_DIM = A_DIM + B_DIM

C_HEAD_DIM = 64
C_HEADS = D_MODEL // C_HEAD_DIM
WIN_R = 8
WIN_C = 16
Q_COL_BLOCK = 16
KEY_COL_SPAN = 32

N_EXPERTS = 16
EXPERT_FF = 2048
CAPACITY_FACTOR = 2

kernel_name = 'hybrid_diffusion_trunk'


def rms_norm(x, w):
    xf = x.astype(jnp.float32)
    y = xf * lax.rsqrt(jnp.mean(xf * xf, axis=-1, keepdims=True) + NORM_EPS)
    return (y * w).astype(x.dtype)


def modulate(xn, shift, scale):
    return xn * (1.0 + scale) + shift


def axial_rope(T, dtype):
    t = jnp.arange(T)
    row = (t // GRID_W).astype(jnp.float32)
    col = (t % GRID_W).astype(jnp.float32)
    n_freq = QK_ROPE // 4
    inv = ROPE_BASE ** (-jnp.arange(n_freq, dtype=jnp.float32) / n_freq)
    ang = jnp.concatenate([row[:, None] * inv, col[:, None] * inv], axis=-1)
    return jnp.cos(ang).astype(dtype), jnp.sin(ang).astype(dtype)


def apply_rope(x, cos, sin):
    x2 = x.reshape(x.shape[:-1] + (-1, 2))
    x0, x1 = x2[..., 0], x2[..., 1]
    return jnp.stack([x0 * cos - x1 * sin, x0 * sin + x1 * cos], axis=-1).reshape(x.shape)


def blocked_attention(q, k, v, scale):
    B, S, H, Dq = q.shape
    Dv = v.shape[-1]
    nb = S // Q_BLOCK
    qb = jnp.moveaxis(q.reshape(B, nb, Q_BLOCK, H, Dq), 1, 0)

    def one_block(q_blk):
        s = jnp.einsum('bqhd,bkhd->bhqk', q_blk, k).astype(jnp.float32) * scale
        p = jax.nn.softmax(s, axis=-1).astype(v.dtype)
        return jnp.einsum('bhqk,bkhd->bqhd', p, v)

    o = lax.map(one_block, qb)
    return jnp.moveaxis(o, 0, 1).reshape(B, S, H, Dv)


def token_shift(f, mu):
    prev = jnp.pad(f, ((0, 0), (1, 0), (0, 0)))[:, :-1]
    nxt = jnp.pad(f, ((0, 0), (0, 1), (0, 0)))[:, 1:]
    return f + mu[0] * (prev - f) + mu[1] * (nxt - f)


def rwkv_prepare(z, mu, w0, w2, a0, a2, g2, k_k, k_a):
    z = token_shift(z, mu).astype(jnp.float32)
    B, T, _ = z.shape
    cuts = [A_DIM, 2 * A_DIM, 3 * A_DIM, 3 * A_DIM + 2 * LORA_W, 3 * A_DIM + 2 * LORA_W + 2 * LORA_A]
    r, k, v, wd, ad, gd = jnp.split(z, cuts, axis=-1)
    wd = wd.reshape(B, T, 2, LORA_W)
    ad = ad.reshape(B, T, 2, LORA_A)
    w_log = -jax.nn.softplus(-(w0 + jnp.einsum('btdr,drc->btdc', jnp.tanh(wd), w2))) - 0.5
    decay = jnp.exp(-jnp.exp(w_log))
    a = jax.nn.sigmoid(a0 + jnp.einsum('btdr,drc->btdc', ad, a2))
    g = jax.nn.sigmoid(gd) @ g2
    kk = (k * k_k).reshape(B, T, A_HEADS, A_HEAD_DIM)
    kk = kk / jnp.maximum(jnp.sqrt(jnp.sum(kk * kk, axis=-1, keepdims=True)), 1e-12)
    kd = k[:, :, None, :] * (1.0 + (a - 1.0) * k_a)
    hd = (B, T, A_HEADS, A_HEAD_DIM)
    hd2 = (B, T, 2, A_HEADS, A_HEAD_DIM)
    return (r.reshape(hd), v.reshape(hd), kk, g, decay.reshape(hd2), a.reshape(hd2), kd.reshape(hd2))


def wkv_scan(state0, r, decay, k, v, kk, a, reverse):
    def step(state, inp):
        r_t, w_t, k_t, v_t, kk_t, a_t = inp
        sa = jnp.einsum('bhvk,bhk->bhv', state, -kk_t)
        state = (state * w_t[:, :, None, :]
                 + sa[..., None] * (kk_t * a_t)[:, :, None, :]
                 + v_t[..., None] * k_t[:, :, None, :])
        return state, jnp.einsum('bhvk,bhk->bhv', state, r_t)

    xs = tuple(jnp.swapaxes(t, 0, 1) for t in (r, decay, k, v, kk, a))
    state, ys = lax.scan(step, state0, xs, reverse=reverse)
    return state, jnp.swapaxes(ys, 0, 1)


def rwkv_scan_dir(state0, feats, d, reverse):
    r, v, kk, g, decay, a, kd = feats
    return wkv_scan(state0, r, decay[:, :, d], kd[:, :, d], v, kk, a[:, :, d], reverse)


def rwkv_output(y, feats, r_k, ln_w, ln_b):
    r, v, kk, g, decay, a, kd = feats
    B, T = y.shape[:2]
    mean = jnp.mean(y, axis=-1, keepdims=True)
    var = jnp.mean(jnp.square(y - mean), axis=-1, keepdims=True)
    yn = (y - mean) * lax.rsqrt(var + GN_EPS)
    yn = yn * ln_w.reshape(A_HEADS, A_HEAD_DIM) + ln_b.reshape(A_HEADS, A_HEAD_DIM)
    bonus = jnp.sum(jnp.sum(r[:, :, None] * kd * r_k, axis=-1, keepdims=True) * v[:, :, None], axis=2)
    return (yn + bonus).reshape(B, T, A_DIM) * g


def mla_project(zq, zkv, zr, cos, sin, qn_w, w_qup, kvn_w, w_kvup):
    B, T, _ = zq.shape
    q = (rms_norm(zq, qn_w) @ w_qup).reshape(B, T, B_HEADS, QK_NOPE + QK_ROPE)
    kv = (rms_norm(zkv, kvn_w) @ w_kvup).reshape(B, T, B_HEADS, QK_NOPE + V_HEAD)
    q_nope, q_rope = q[..., :QK_NOPE], q[..., QK_NOPE:]
    k_nope, v = kv[..., :QK_NOPE], kv[..., QK_NOPE:]
    k_rope = zr
    if cos is not None:
        q_rope = apply_rope(q_rope, cos[:, None], sin[:, None])
        k_rope = apply_rope(k_rope, cos, sin)
    k_full = jnp.concatenate([k_nope, jnp.broadcast_to(k_rope[:, :, None, :], (B, T, B_HEADS, QK_ROPE))], axis=-1)
    q_full = jnp.concatenate([q_nope, q_rope], axis=-1)
    return q_full, k_full, v


def rwkv_mla_mixer(h, hc, cos, sin, need_ctx, w_in, w_out, mu, w0, w2, a0, a2, g2, k_k, k_a, r_k,
                   ln_w, ln_b, qn_w, w_qup, kvn_w, w_kvup):
    B, S, _ = h.shape
    L = hc.shape[1]
    cuts = [A_COLS, A_COLS + Q_RANK, A_COLS + Q_RANK + KV_RANK]
    za, zq, zkv, zr = jnp.split(h @ w_in, cuts, axis=-1)
    zac, zqc, zkvc, zrc = jnp.split(hc @ w_in, cuts, axis=-1)
    lat = rwkv_prepare(za, mu, w0, w2, a0, a2, g2, k_k, k_a)
    cx = rwkv_prepare(zac, mu, w0, w2, a0, a2, g2, k_k, k_a)
    state0 = jnp.zeros((B, A_HEADS, A_HEAD_DIM, A_HEAD_DIM), jnp.float32)
    st_f, yc_f = rwkv_scan_dir(state0, cx, 0, False)
    st_b, yc_b = rwkv_scan_dir(state0, cx, 1, True)
    _, y_f = rwkv_scan_dir(st_f, lat, 0, False)
    _, y_b = rwkv_scan_dir(st_b, lat, 1, True)
    o_a = rwkv_output(y_f + y_b, lat, r_k, ln_w, ln_b).astype(h.dtype)
    q, k, v = mla_project(zq, zkv, zr, cos, sin, qn_w, w_qup, kvn_w, w_kvup)
    qc, kc, vc = mla_project(zqc, zkvc, zrc, None, None, qn_w, w_qup, kvn_w, w_kvup)
    scale = (QK_NOPE + QK_ROPE) ** -0.5
    o_b = blocked_attention(q, jnp.concatenate([kc, k], axis=1), jnp.concatenate([vc, v], axis=1), scale)
    out = jnp.concatenate([o_a, o_b.reshape(B, S, B_DIM)], axis=-1) @ w_out
    if not need_ctx:
        return out, None
    oc_a = rwkv_output(yc_f + yc_b, cx, r_k, ln_w, ln_b).astype(hc.dtype)
    oc_b = blocked_attention(qc, kc, vc, scale)
    out_c = jnp.concatenate([oc_a, oc_b.reshape(B, L, B_DIM)], axis=-1) @ w_out
    return out, out_c


def neighbourhood_attention(q, k, v, k_ctx, v_ctx, rpb):
    B, S, H, Dh = q.shape
    rows = S // GRID_W
    kr = min(WIN_R, rows)
    n_cb = GRID_W // Q_COL_BLOCK
    scale = Dh ** -0.5
    qrow = jnp.arange(rows)
    row_start = jnp.clip(qrow - kr // 2, 0, rows - kr)
    key_rows = row_start[:, None] + jnp.arange(kr)
    dr_idx = key_rows - qrow[:, None] + (WIN_R - 1)
    cb = jnp.arange(n_cb)
    qcol = cb[:, None] * Q_COL_BLOCK + jnp.arange(Q_COL_BLOCK)
    col_start = jnp.clip(qcol - WIN_C // 2, 0, GRID_W - WIN_C)
    span_start = jnp.clip(cb * Q_COL_BLOCK - WIN_C // 2, 0, GRID_W - KEY_COL_SPAN)
    key_cols = span_start[:, None] + jnp.arange(KEY_COL_SPAN)
    kcb = key_cols[:, None, :]
    col_valid = (kcb >= col_start[..., None]) & (kcb < col_start[..., None] + WIN_C)
    dc_idx = jnp.clip(kcb - qcol[..., None] + (WIN_C - 1), 0, 2 * WIN_C - 2)
    kg = k.reshape(B, rows, GRID_W, H, Dh)
    vg = v.reshape(B, rows, GRID_W, H, Dh)
    qg = jnp.moveaxis(q.reshape(B, rows, n_cb, Q_COL_BLOCK, H, Dh), 1, 0)
    n_nb = kr * KEY_COL_SPAN

    def one_row(args):
        q_r, rows_r, dr_r = args
        k_blk = kg[:, rows_r][:, :, key_cols]
        v_blk = vg[:, rows_r][:, :, key_cols]
        s_nb = jnp.einsum('bnqhd,brnkhd->bhnqrk', q_r, k_blk).astype(jnp.float32) * scale
        bias = jnp.transpose(rpb[:, dr_r][:, :, dc_idx], (0, 2, 3, 1, 4)).astype(jnp.float32)
        s_nb = jnp.where(col_valid[:, :, None, :], s_nb + bias, NEG_INF)
        s_ctx = jnp.einsum('bnqhd,blhd->bhnql', q_r, k_ctx).astype(jnp.float32) * scale
        s = jnp.concatenate([s_nb.reshape(s_nb.shape[:4] + (n_nb,)), s_ctx], axis=-1)
        p = jax.nn.softmax(s, axis=-1).astype(v.dtype)
        p_nb = p[..., :n_nb].reshape(s_nb.shape)
        p_ctx = p[..., n_nb:]
        return (jnp.einsum('bhnqrk,brnkhd->bnqhd', p_nb, v_blk)
                + jnp.einsum('bhnql,blhd->bnqhd', p_ctx, v_ctx))

    out = lax.map(one_row, (qg, key_rows, dr_idx))
    return jnp.moveaxis(out, 0, 1).reshape(B, S, H, Dh)


def na_mixer(h, hc, need_ctx, w_qkv, rpb, w_out):
    B, S, D = h.shape
    L = hc.shape[1]
    qkv = (h @ w_qkv).reshape(B, S, 3, C_HEADS, C_HEAD_DIM)
    kvc = (hc @ w_qkv[:, D:]).reshape(B, L, 2, C_HEADS, C_HEAD_DIM)
    kc, vc = kvc[:, :, 0], kvc[:, :, 1]
    o = neighbourhood_attention(qkv[:, :, 0], qkv[:, :, 1], qkv[:, :, 2], kc, vc, rpb)
    out = o.reshape(B, S, D) @ w_out
    if not need_ctx:
        return out, None
    qc = (hc @ w_qkv[:, :D]).reshape(B, L, C_HEADS, C_HEAD_DIM)
    oc = blocked_attention(qc, kc, vc, C_HEAD_DIM ** -0.5)
    return out, oc.reshape(B, L, D) @ w_out


def expert_choice_ffn(h, w_router, w1, w3, w2):
    B, T, D = h.shape
    cap = CAPACITY_FACTOR * T // N_EXPERTS
    aff = jax.nn.softmax((h @ w_router).astype(jnp.float32), axis=-1)
    gate, idx = lax.top_k(jnp.swapaxes(aff, 1, 2), cap)
    xin = jax.vmap(lambda hb, ib: hb[ib])(h, idx)
    hid = jax.nn.silu(jnp.einsum('becd,edf->becf', xin, w1)) * jnp.einsum('becd,edf->becf', xin, w3)
    y = jnp.einsum('becf,efd->becd', hid, w2) * gate[..., None].astype(h.dtype)
    return jax.vmap(lambda yb, ib: jnp.zeros((T, D), yb.dtype).at[ib.reshape(-1)].add(yb.reshape(-1, D)))(y, idx)


def setup_inputs(seed: int = 0) -> dict:
    key = jax.random.key(seed)
    ks = iter(jax.random.split(key, 64))
    n_even = (DEPTH + 1) // 2
    n_odd = DEPTH // 2
    D = D_MODEL

    def nrm(shape, scale):
        return jax.random.normal(next(ks), shape, jnp.float32) * scale

    def unif(shape, lo, hi):
        return jax.random.uniform(next(ks), shape, jnp.float32, lo, hi)

    return {
        'x': nrm((BATCH, SEQ, D), 1.0),
        'c': nrm((BATCH, D), 1.0),
        'ctx': nrm((BATCH, CTX_LEN, D), 1.0),
        'c_ctx': nrm((D,), 1.0),
        'mod_w': nrm((DEPTH, D, 6 * D), 0.5 * D ** -0.5),
        'mod_b': nrm((DEPTH, 6 * D), 0.02),
        'norm1_w': 1.0 + nrm((DEPTH, D), 0.02),
        'norm2_w': 1.0 + nrm((DEPTH, D), 0.02),
        'final_norm_w': 1.0 + nrm((D,), 0.02),
        'ab_w_in': nrm((n_even, D, AB_COLS), D ** -0.5),
        'ab_w_out': nrm((n_even, MIX_DIM, D), MIX_DIM ** -0.5),
        'rk_mu': unif((n_even, 2, A_COLS), 0.0, 0.5),
        'rk_w0': unif((n_even, 2, A_DIM), -6.5, -1.5),
        'rk_w2': nrm((n_even, 2, LORA_W, A_DIM), 0.5 * LORA_W ** -0.5),
        'rk_a0': nrm((n_even, 2, A_DIM), 0.1),
        'rk_a2': nrm((n_even, 2, LORA_A, A_DIM), 0.5 * LORA_A ** -0.5),
        'rk_g2': nrm((n_even, LORA_G, A_DIM), LORA_G ** -0.5),
        'rk_kk': 0.85 + nrm((n_even, A_DIM), 0.02),
        'rk_ka': 1.0 + nrm((n_even, A_DIM), 0.02),
        'rk_rk': nrm((n_even, A_HEADS, A_HEAD_DIM), 0.1),
        'rk_ln_w': 1.0 + nrm((n_even, A_DIM), 0.02),
        'rk_ln_b': nrm((n_even, A_DIM), 0.02),
        'mla_qn_w': 1.0 + nrm((n_even, Q_RANK), 0.02),
        'mla_w_qup': nrm((n_even, Q_RANK, B_HEADS * (QK_NOPE + QK_ROPE)), Q_RANK ** -0.5),
        'mla_kvn_w': 1.0 + nrm((n_even, KV_RANK), 0.02),
        'mla_w_kvup': nrm((n_even, KV_RANK, B_HEADS * (QK_NOPE + V_HEAD)), KV_RANK ** -0.5),
        'na_w_qkv': nrm((n_odd, D, 3 * D), D ** -0.5),
        'na_rpb': nrm((n_odd, C_HEADS, 2 * WIN_R - 1, 2 * WIN_C - 1), 0.1),
        'na_w_out': nrm((n_odd, D, D), D ** -0.5),
        'moe_router': nrm((DEPTH, D, N_EXPERTS), D ** -0.5),
        'moe_w1': nrm((DEPTH, N_EXPERTS, D, EXPERT_FF), D ** -0.5),
        'moe_w3': nrm((DEPTH, N_EXPERTS, D, EXPERT_FF), D ** -0.5),
        'moe_w2': nrm((DEPTH, N_EXPERTS, EXPERT_FF, D), EXPERT_FF ** -0.5),
    }


def reference(x, c, ctx, c_ctx, mod_w, mod_b, norm1_w, norm2_w, final_norm_w, ab_w_in, ab_w_out,
              rk_mu, rk_w0, rk_w2, rk_a0, rk_a2, rk_g2, rk_kk, rk_ka, rk_rk, rk_ln_w, rk_ln_b,
              mla_qn_w, mla_w_qup, mla_kvn_w, mla_w_kvup, na_w_qkv, na_rpb, na_w_out,
              moe_router, moe_w1, moe_w3, moe_w2):
    S = x.shape[1]
    cos, sin = axial_rope(S, x.dtype)
    silu_c = jax.nn.silu(c)
    silu_cc = jax.nn.silu(c_ctx)
    for layer in range(DEPTH):
        need_ctx = layer < DEPTH - 1
        i = layer // 2
        m = (silu_c @ mod_w[layer] + mod_b[layer])[:, None, :]
        sh1, sc1, g1, sh2, sc2, g2 = jnp.split(m, 6, axis=-1)
        mc = silu_cc @ mod_w[layer] + mod_b[layer]
        sh1c, sc1c, g1c, sh2c, sc2c, g2c = jnp.split(mc, 6, axis=-1)
        h = modulate(rms_norm(x, norm1_w[layer]), sh1, sc1)
        hc = modulate(rms_norm(ctx, norm1_w[layer]), sh1c, sc1c)
        if layer % 2 == 0:
            o, oc = rwkv_mla_mixer(h, hc, cos, sin, need_ctx, ab_w_in[i], ab_w_out[i], rk_mu[i], rk_w0[i],
                                   rk_w2[i], rk_a0[i], rk_a2[i], rk_g2[i], rk_kk[i], rk_ka[i], rk_rk[i],
                                   rk_ln_w[i], rk_ln_b[i], mla_qn_w[i], mla_w_qup[i], mla_kvn_w[i], mla_w_kvup[i])
        else:
            o, oc = na_mixer(h, hc, need_ctx, na_w_qkv[i], na_rpb[i], na_w_out[i])
        x = x + g1 * o
        h2 = modulate(rms_norm(x, norm2_w[layer]), sh2, sc2)
        x = x + g2 * expert_choice_ffn(h2, moe_router[layer], moe_w1[layer], moe_w3[layer], moe_w2[layer])
        if need_ctx:
            ctx = ctx + g1c * oc
            h2c = modulate(rms_norm(ctx, norm2_w[layer]), sh2c, sc2c)
            ctx = ctx + g2c * expert_choice_ffn(h2c, moe_router[layer], moe_w1[layer], moe_w3[layer], moe_w2[layer])
    return rms_norm(x, final_norm_w)
```

```python
from concourse.bass_utils import run_bass_kernel_spmd
import numpy as np
import concourse.bass as bass
import concourse.mybir as mybir

F32 = mybir.dt.float32
BF16 = mybir.dt.bfloat16
AF = mybir.ActivationFunctionType
ALU = mybir.AluOpType
AX = mybir.AxisListType

ENGS = ("pe", "act", "dve", "pool", "sp")


class Buf:
    def __init__(self, t, name, root=None, excl=False):
        self.t = t
        self.name = name
        self.root = root if root is not None else self
        if root is None:
            self._w = None
            self._r = {}
            self.excl = excl
        else:
            self.excl = root.excl

    @property
    def w(self):
        return self.root._w

    @w.setter
    def w(self, v):
        self.root._w = v

    @property
    def r(self):
        return self.root._r

    @r.setter
    def r(self, v):
        self.root._r = v

    def __getitem__(self, idx):
        return (self, self.t[idx])


class KB:
    def __init__(self, nc, stack, n_dma_sems=8):
        self.nc = nc
        self.stack = stack
        self.prog = {e: [] for e in ENGS}
        self.cnt = {e: 0 for e in ENGS}
        self.sem = {}
        for e in ENGS:
            self.sem[e] = stack.enter_context(nc.semaphore("s_" + e))
        self.seen = {e: {} for e in ENGS}
        self.nring = n_dma_sems
        self.ring = {}
        self.ring_i = {}
        for q in ("sp", "pool", "act"):
            self.ring[q] = [stack.enter_context(nc.semaphore(f"d_{q}_{i}")) for i in range(n_dma_sems)]
            self.ring_i[q] = 0
        self.semobj = {}
        for e in ENGS:
            self.semobj[("e", e)] = self.sem[e]
        for q in self.ring:
            for i, s in enumerate(self.ring[q]):
                self.semobj[("d", q, i)] = s
        self.nbuf = 0

    def sb(self, shape, dt=F32, name=None):
        self.nbuf += 1
        name = f"sb{self.nbuf}_{name or ''}"
        t = self.stack.enter_context(self.nc.sbuf_tensor(name, list(shape), dt))
        return Buf(t, name)

    def ps(self, shape, dt=F32, name=None):
        self.nbuf += 1
        name = f"ps{self.nbuf}_{name or ''}"
        t = self.stack.enter_context(self.nc.psum_tensor(name, list(shape), dt))
        return Buf(t, name, excl=True)

    def dram(self, name, shape, dt=F32, kind="Internal"):
        t = self.nc.dram_tensor(name, list(shape), dt, kind=kind)
        return Buf(t, name)

    def _wait(self, eng, ev):
        if ev is None:
            return
        key, val = ev
        if self.seen[eng].get(key, 0) >= val:
            return
        self.seen[eng][key] = val
        sem = self.semobj[key]
        self.prog[eng].append(lambda E, sem=sem, val=val: E.wait_ge(sem, val))

    def _deps(self, eng, reads, writes):
        for b in reads:
            self._wait(eng, b.w)
        for b in writes:
            self._wait(eng, b.w)
            for k, v in b.r.items():
                self._wait(eng, (k, v))

    def _mark(self, ev, reads, writes):
        for b in reads:
            k, v = ev
            if b.r.get(k, 0) < v:
                b.r[k] = v
        for b in writes:
            b.w = ev
            b.r = {}

    stopped = False

    def op(self, eng, fn, reads, writes):
        if self.stopped:
            return None
        ex = [b for b in reads if b.excl]
        if ex:
            reads = [b for b in reads if not b.excl]
            writes = list(writes) + ex
        self._deps(eng, reads, writes)
        self.cnt[eng] += 1
        n = self.cnt[eng]
        sem = self.sem[eng]
        self.prog[eng].append(lambda E, fn=fn, sem=sem: fn(E).then_inc(sem, 1))
        ev = (("e", eng), n)
        self.seen[eng][("e", eng)] = max(self.seen[eng].get(("e", eng), 0), 0)
        self._mark(ev, reads, writes)
        return ev

    def dma(self, out, in_, q="sp", **kw):
        if self.stopped:
            return None
        ob, oap = out
        ib, iap = in_
        self._deps(q, [ib], [ob])
        i = self.ring_i[q]
        self.ring_i[q] += 1
        slot = i % self.nring
        use = i // self.nring
        key = ("d", q, slot)
        if use > 0:
            self._wait(q, (key, 16 * use))
        sem = self.semobj[key]
        self.prog[q].append(lambda E, oap=oap, iap=iap, sem=sem, kw=kw: E.dma_start(out=oap, in_=iap, **kw).then_inc(sem, 16))
        ev = (key, 16 * (use + 1))
        self._mark(ev, [ib], [ob])
        return ev

    def mm(self, out, lhsT, rhs, start=True, stop=True):
        ob, oap = out
        lb, lap = lhsT
        rb, rap = rhs
        return self.op("pe", lambda E: E.matmul(oap, lap, rap, start=start, stop=stop), [lb, rb], [ob])

    def tr(self, out, in_, ident):
        ob, oap = out
        ib, iap = in_
        idb, idap = ident
        return self.op("pe", lambda E: E.transpose(oap, iap, idap), [ib, idb], [ob])

    def act(self, out, in_, func, bias=None, scale=None, accum=None, eng="act"):
        ob, oap = out
        ib, iap = in_
        reads = [ib]
        writes = [ob]
        kw = {}
        if bias is not None:
            if isinstance(bias, tuple):
                reads.append(bias[0]); kw["bias"] = bias[1]
            else:
                kw["bias"] = bias
        if scale is not None:
            if isinstance(scale, tuple):
                reads.append(scale[0]); kw["scale"] = scale[1]
            else:
                kw["scale"] = scale
        if accum is not None:
            writes.append(accum[0]); kw["accum_out"] = accum[1]
        return self.op(eng, lambda E: E.activation(oap, iap, func, **kw), reads, writes)

    def tt(self, out, a, b, op, eng="dve"):
        ob, oap = out
        return self.op(eng, lambda E: E.tensor_tensor(oap, a[1], b[1], op), [a[0], b[0]], [ob])

    def ts(self, out, a, s1, s2, op0, op1=None, eng="dve", accum=None):
        ob, oap = out
        reads = [a[0]]
        writes = [ob]
        v1 = s1
        v2 = s2
        if isinstance(s1, tuple):
            reads.append(s1[0]); v1 = s1[1]
        if isinstance(s2, tuple):
            reads.append(s2[0]); v2 = s2[1]
        kw = {}
        if op1 is not None:
            kw["op1"] = op1
        if accum is not None:
            writes.append(accum[0]); kw["accum_out"] = accum[1]
        return self.op(eng, lambda E: E.tensor_scalar(oap, a[1], v1, v2, op0, **kw), reads, writes)

    def stt(self, out, a, s, b, op0, op1, eng="dve"):
        ob, oap = out
        reads = [a[0], b[0]]
        v = s
        if isinstance(s, tuple):
            reads.append(s[0]); v = s[1]
        return self.op(eng, lambda E: E.scalar_tensor_tensor(oap, a[1], v, b[1], op0, op1), reads, [ob])

    def copy(self, out, in_, eng="dve"):
        ob, oap = out
        ib, iap = in_
        if eng == "act":
            return self.op("act", lambda E: E.copy(oap, iap), [ib], [ob])
        return self.op(eng, lambda E: E.tensor_copy(oap, iap), [ib], [ob])

    def memset(self, out, val, eng="dve"):
        ob, oap = out
        return self.op(eng, lambda E: E.memset(oap, val), [], [ob])

    def reduce(self, out, in_, op, axis=AX.X, eng="dve"):
        ob, oap = out
        ib, iap = in_
        return self.op(eng, lambda E: E.tensor_reduce(oap, iap, axis, op), [ib], [ob])

    def recip(self, out, in_):
        ob, oap = out
        ib, iap = in_
        return self.op("dve", lambda E: E.reciprocal(oap, iap), [ib], [ob])

    def all_events(self):
        evs = [(("e", e), self.cnt[e]) for e in ENGS if self.cnt[e] > 0]
        for q in self.ring:
            n = self.ring_i[q]
            for slot in range(min(n, self.nring)):
                uses = (n - slot + self.nring - 1) // self.nring
                evs.append((("d", q, slot), 16 * uses))
        if ("c",) in self.semobj and getattr(self, "ccn", 0) > 0:
            evs.append((("c",), self.ccn))
        return evs

    def emit_block(self, final=False):
        evs = self.all_events()
        for e in ENGS:
            for ev in evs:
                self._wait(e, ev)
        nc = self.nc
        prog = self.prog
        with nc.Block() as block:
            @block.tensor
            def _(E):
                for f in prog["pe"]:
                    f(E)

            @block.scalar
            def _(E):
                for f in prog["act"]:
                    f(E)

            @block.vector
            def _(E):
                for f in prog["dve"]:
                    f(E)

            @block.gpsimd
            def _(E):
                for f in prog["pool"]:
                    f(E)

            @block.sync
            def _(E):
                for f in prog["sp"]:
                    f(E)
        self.prog = {e: [] for e in ENGS}

    def emit(self, final_events=()):
        self.emit_block(final=True)


def _kb_cc(self, kind, out, in_, groups, inc=1):
    q = "pool"
    ob, oap = out
    ib, iap = in_
    if ("c",) not in self.semobj:
        self.semobj[("c",)] = self.stack.enter_context(self.nc.semaphore("cc_sem"))
        self.ccn = 0
    self._deps(q, [ib], [ob])
    key = ("c",)
    if self.ccn > 0:
        self._wait(q, (key, inc * self.ccn))
    self.ccn += 1
    sem = self.semobj[key]
    if inc == 1:
        self.prog[q].append(lambda E: E.collective_compute(kind, ALU.bypass, replica_groups=groups, ins=[iap.opt()], outs=[oap.opt()]).then_inc(sem))
    else:
        self.prog[q].append(lambda E: E.collective_compute(kind, ALU.bypass, replica_groups=groups, ins=[iap.opt()], outs=[oap.opt()]).then_inc(sem, inc))
    ev = (key, inc * self.ccn)
    self._mark(ev, [ib], [ob])
    return ev


KB.cc = _kb_cc


from contextlib import contextmanager, ExitStack

T = 2304
NT = 18
LC = 256
D = 1024
EPS = 1e-6
BLKS = [(0, 512), (512, 512), (1024, 512), (1536, 512), (2048, 256)]


class Rot:
    def __init__(self, bufs):
        self.b = bufs
        self.i = 0

    def next(self):
        x = self.b[self.i % len(self.b)]
        self.i += 1
        return x


class StopBuild(Exception):
    pass


import os
STOP = int(os.environ.get("KSTOP", "0"))


KBREF = [None]


def ckpt(k):
    if STOP == k and KBREF[0] is not None:
        KBREF[0].stopped = True


@contextmanager
def scope(kb):
    old = kb.stack
    with ExitStack() as st:
        kb.stack = st
        try:
            yield
        except StopBuild:
            kb.emit_block()
            kb.stack = old
            raise
        kb.emit_block()
    kb.stack = old


def colload(kb, C, dst, src_rows_ap, R, pt=None):
    tmp = kb.sb([R, 128], F32)
    kb.dma(tmp[:], src_rows_ap)
    p = pt if pt is not None else kb.ps([128, 512], F32)
    kb.tr(p[:, 0:R], tmp[:], C["ident"][0:R, 0:R])
    kb.copy(dst, p[:, 0:R])


def load_consts(kb, cin):
    C = {}
    C["ident"] = kb.sb([128, 128], F32, "c_ident")
    kb.dma(C["ident"][:], cin["ident"][:])
    C["identb"] = kb.sb([128, 128], BF16, "c_identb")
    kb.copy(C["identb"][:], C["ident"][:])
    C["ones"] = kb.sb([128, 128], F32, "c_ones")
    kb.memset(C["ones"][:], 1.0)
    C["blk64"] = kb.sb([128, 128], F32, "c_blk64")
    kb.dma(C["blk64"][:], cin["blk64"][:])
    return C


def ph_mod(kb, C, c2, mod_w, mod_b, n1w, n2w, modv):
    with scope(kb):
        c2s = kb.sb([16, 128], F32)
        kb.dma(c2s[:], c2[:])
        kb.act(c2s[:], c2s[:], AF.Silu)
        pt = kb.ps([128, 16], F32)
        kb.tr(pt[:], c2s[:], C["ident"][0:16, 0:16])
        scT = kb.sb([128, 16], F32)
        kb.copy(scT[:], pt[:])
        modb2 = kb.sb([2, 6144], F32)
        kb.dma(modb2[:], (mod_b, mod_b.t.ap().partition_broadcast(2)))
        msb = kb.sb([2, 6144], F32)
        wr = Rot([kb.sb([128, 8, 512], F32) for _ in range(2)])
        pr = Rot([kb.ps([2, 512], F32) for _ in range(2)])
        mw = mod_w.t.ap().rearrange("(c p) n -> p c n", p=128)
        for blk in range(12):
            wb = wr.next()
            kb.dma(wb[:], (mod_w, mw[:, :, blk * 512:(blk + 1) * 512]), q=("sp" if blk % 2 == 0 else "act"))
            pm = pr.next()
            for k in range(8):
                kb.mm(pm[:], scT[:, k:16:8], wb[:, k, :], start=(k == 0), stop=(k == 7))
            kb.tt(msb[:, blk * 512:(blk + 1) * 512], pm[:], modb2[:, blk * 512:(blk + 1) * 512], ALU.add)
        nw = kb.sb([2, 2, 1024], F32)
        kb.dma(nw[:, 0, :], (n1w, n1w.t.ap().partition_broadcast(2)))
        kb.dma(nw[:, 1, :], (n2w, n2w.t.ap().partition_broadcast(2)))
        kb.stt(msb[:, 1024:2048], msb[:, 1024:2048], 1.0, nw[:, 0, :], ALU.add, ALU.mult)
        kb.stt(msb[:, 4096:5120], msb[:, 4096:5120], 1.0, nw[:, 1, :], ALU.add, ALU.mult)
        kb.dma(modv[:], msb[:])


def bload(kb, modv, row, k, name=None):
    t = kb.sb([128, 1024], F32, name)
    kb.dma(t[:], (modv, modv.t[row:row + 1, k * 1024:(k + 1) * 1024].partition_broadcast(128)))
    return t


def ph_norm(kb, C, src, modv, ksh, keff, hT=None, hTM=None, hTf=None, tiles=range(NT)):
    effb = [bload(kb, modv, 1, keff), bload(kb, modv, 0, keff)]
    shb = [bload(kb, modv, 1, ksh), bload(kb, modv, 0, ksh)]
    xr = Rot([kb.sb([128, 1024], F32) for _ in range(2)])
    hfr = Rot([kb.sb([128, 1024], F32) for _ in range(2)])
    hbr = Rot([kb.sb([128, 1024], BF16) for _ in range(2)])
    junk = kb.sb([128, 1024], F32)
    st = Rot([kb.sb([128, 2], F32) for _ in range(2)])
    ptr = Rot([kb.ps([128, 4, 128], BF16) for _ in range(2)]) if hT is not None else None
    for i in tiles:
        w = 0 if i < 2 else 1
        xt = xr.next()
        kb.dma(xt[:], src[i * 128:(i + 1) * 128, :], q=("sp" if i % 2 == 0 else "act"))
        s = st.next()
        kb.memset(s[:], 0.0)
        kb.act(junk[:], xt[:], AF.Square, accum=s[:, 0:1])
        kb.ts(s[:, 1:2], s[:, 0:1], 1.0 / 1024, EPS, ALU.mult, ALU.add)
        kb.act(s[:, 1:2], s[:, 1:2], AF.Sqrt)
        kb.recip(s[:, 1:2], s[:, 1:2])
        hf = hfr.next()
        kb.stt(hf[:], xt[:], s[:, 1:2], effb[w][:], ALU.mult, ALU.mult)
        kb.tt(hf[:], hf[:], shb[w][:], ALU.add)
        if hTf is not None:
            hTf(i, hf)
        if hTM is not None:
            kb.copy(hTM[:, i, :], hf[:], eng="act")
        if hT is not None:
            hb = hbr.next()
            kb.copy(hb[:], hf[:], eng="act")
            for g in range(2):
                pt = ptr.next()
                for c in range(4):
                    kb.tr(pt[:, c, :], hb[:, (g * 4 + c) * 128:(g * 4 + c + 1) * 128], C["identb"][:])
                kb.copy(hT[:, g * 4:(g + 1) * 4, i * 128:(i + 1) * 128], pt[:], eng=("dve" if g == 0 else "act"))


def proj_fm(kb, w_sb, col0, M, hT, nk, dst_fn, pr, evac_alt=[0]):
    for (lo, n) in BLKS:
        p = pr.next()
        for k in range(nk):
            kb.mm(p[0:M, 0:n], w_sb[:, k, col0:col0 + M], hT[:, k, lo:lo + n], start=(k == 0), stop=(k == nk - 1))
        evac_alt[0] += 1
        kb.copy(dst_fn(lo, n), p[0:M, 0:n], eng=("act" if evac_alt[0] % 2 else "dve"))


def ph_inproj(kb, C, src, modv, w_in, mu, zsT):
    with scope(kb):
        hT = kb.sb([128, 8, T], BF16, "hT")
        with scope(kb):
            ph_norm(kb, C, src, modv, 0, 1, hT=hT)
        win = kb.sb([128, 8, 2592], BF16, "win")
        wv = w_in.t.ap().rearrange("(c p) n -> p c n", p=128)
        for k in range(8):
            kb.dma(win[:, k, :], (w_in, wv[:, k, :]), q="pool")
        muT = kb.sb([128, 30], F32)
        colload(kb, C, muT[:], (mu, mu.t.ap().rearrange("a (c p) -> (a c) p", p=128)), 30)
        coef = kb.sb([128, 15], F32)
        kb.tt(coef[:], muT[:, 0:15], muT[:, 15:30], ALU.add)
        kb.ts(coef[:], coef[:], -1.0, 1.0, ALU.mult, ALU.add)
        zr_ = Rot([kb.sb([128, T], F32) for _ in range(2)])
        zsr = Rot([kb.sb([128, T], F32) for _ in range(2)])
        pr = Rot([kb.ps([128, 512], F32) for _ in range(3)])
        for ch in range(21):
            M = 128 if ch < 20 else 32
            z = zr_.next()
            proj_fm(kb, win, ch * 128, M, hT, 8, lambda lo, n, z=z, M=M: z[0:M, lo:lo + n], pr)
            if ch < 15:
                zs = zsr.next()
                m0 = muT[:, ch:ch + 1]
                m1 = muT[:, 15 + ch:16 + ch]
                kb.ts(zs[:], z[:], coef[:, ch:ch + 1], None, ALU.mult)
                kb.stt(zs[:, 1:LC], z[:, 0:LC - 1], m0, zs[:, 1:LC], ALU.mult, ALU.add)
                kb.stt(zs[:, LC + 1:T], z[:, LC:T - 1], m0, zs[:, LC + 1:T], ALU.mult, ALU.add)
                kb.stt(zs[:, 0:LC - 1], z[:, 1:LC], m1, zs[:, 0:LC - 1], ALU.mult, ALU.add)
                kb.stt(zs[:, LC:T - 1], z[:, LC + 1:T], m1, zs[:, LC:T - 1], ALU.mult, ALU.add)
                kb.dma(zsT[ch * 128:(ch + 1) * 128, :], zs[:])
            else:
                kb.dma(zsT[ch * 128:ch * 128 + M, :], z[0:M, :])


def sub(buf, ap, name=None):
    return Buf(ap, name or (buf.name + "_s"), root=buf.root)


CDEC = 0.6065306597126334


def ph_rwkv(kb, C, cin, zsT, W, OTd, ccs=range(4), dirs=(0, 1), dbg=None):
    with scope(kb):
        banks = [kb.ps([128, 512], F32, f"bank{i}") for i in range(8)]
        colsT = kb.sb([128, 28], F32)
        colload(kb, C, colsT[:, 0:8], (W["w0"], W["w0"].t.ap().rearrange("a (c p) -> (a c) p", p=128)), 8, pt=banks[0])
        colload(kb, C, colsT[:, 8:16], (W["a0"], W["a0"].t.ap().rearrange("a (c p) -> (a c) p", p=128)), 8, pt=banks[0])
        colload(kb, C, colsT[:, 16:20], (W["kk"], W["kk"].t.ap().rearrange("(c p) -> c p", p=128)), 4, pt=banks[0])
        colload(kb, C, colsT[:, 20:24], (W["ka"], W["ka"].t.ap().rearrange("(c p) -> c p", p=128)), 4, pt=banks[0])
        colload(kb, C, colsT[:, 24:28], (W["rk"], W["rk"].t.ap().rearrange("(c a) k -> c (a k)", a=2)), 4, pt=banks[0])
        w2s = kb.sb([128, 512], F32)
        kb.dma(w2s[:], (W["w2"], W["w2"].t.ap().rearrange("a r c -> (a r) c")))
        a2s = kb.sb([128, 512], F32)
        kb.dma(a2s[:], (W["a2"], W["a2"].t.ap().rearrange("a r c -> (a r) c")))
        g2s = kb.sb([128, 512], F32)
        kb.dma(g2s[:], W["g2"][:])
        lnw = kb.sb([64, 512], F32)
        kb.dma(lnw[:], (W["lnw"], W["lnw"].t.ap().partition_broadcast(64)))
        lnb = kb.sb([64, 512], F32)
        kb.dma(lnb[:], (W["lnb"], W["lnb"].t.ap().partition_broadcast(64)))
        mseg = kb.sb([128, T], F32)
        kb.dma(mseg[:], cin["mseg"][:])
        maskM = kb.sb([64, 2, 2, 128], F32)
        kb.dma(maskM[:], cin["maskM"][:])
        maskN = kb.sb([64, 2, 2, 64], F32)
        kb.dma(maskN[:], cin["maskN"][:])
        hsel = kb.sb([128, 2], F32)
        kb.dma(hsel[:], cin["hsel"][:])
        twd = kb.sb([128, T], F32)
        kb.dma(twd[:], zsT[1536:1664, :])
        kb.act(twd[:], twd[:], AF.Tanh)
        adf = kb.sb([128, T], F32)
        kb.dma(adf[:], zsT[1664:1792, :])
        prep = Rot([banks[0], banks[1]])
        pMbk = [(sub(banks[2 + i], banks[2 + i].t[0:64, 0:256]), sub(banks[2 + i], banks[2 + i].t[0:64, 256:512])) for i in range(2)]
        pNT = sub(banks[4], banks[4].t[0:64, 0:128])
        pTM = sub(banks[4], banks[4].t[0:64, 128:512])
        pWr = Rot([sub(banks[5], banks[5].t[0:64, 0:256]), sub(banks[5], banks[5].t[0:64, 256:512])])
        pNr = Rot([sub(banks[6], banks[6].t[0:64, 0:256]), sub(banks[6], banks[6].t[0:64, 256:512])])
        pRh = sub(banks[7], banks[7].t[:, 0:64])
        pY = sub(banks[7], banks[7].t[0:64, 64:192])
        pPhi = sub(banks[7], banks[7].t[:, 192:256])
        pPsi = sub(banks[7], banks[7].t[:, 256:320])
        pS = sub(banks[7], banks[7].t[:, 320:384])
        rF = kb.sb([128, T], F32, "rF")
        kF = kb.sb([128, T], F32, "kF")
        kkF = kb.sb([128, T], F32, "kkF")
        vT = kb.sb([64, 36, 128], F32, "vT")
        s1 = kb.sb([128, T], F32, "s1")
        s2 = kb.sb([128, T], F32, "s2")
        s3 = kb.sb([128, T], F32, "s3")
        s4 = kb.sb([128, T], F32, "s4")
        s5 = kb.sb([128, T], F32, "s5")
        AR = kb.sb([128, 36, 128], F32, "AR")
        ysum = kb.sb([64, 36, 128], F32, "ysum")
        coef = kb.sb([64, 2, 36, 2], F32, "coef")
        pCt = kb.sb([128, 36], F32, "pCt")
        tot = kb.sb([128, 36], F32, "tot")
        ST = kb.sb([128, 64], F32, "ST")
        Mbr_ = Rot([kb.sb([64, 2, 128], F32) for _ in range(2)])
        Mkr_ = Rot([kb.sb([64, 2, 128], F32) for _ in range(2)])
        NTr_ = Rot([kb.sb([64, 2, 64], F32) for _ in range(2)])
        TMr_ = Rot([kb.sb([64, 3, 128], F32) for _ in range(2)])
        Wr_ = Rot([kb.sb([64, 2, 128], F32) for _ in range(3)])
        Npr_ = Rot([kb.sb([64, 2, 128], F32) for _ in range(3)])
        Rhr_ = Rot([kb.sb([128, 64], F32) for _ in range(2)])
        Phr_ = Rot([kb.sb([128, 64], F32) for _ in range(2)])
        Psr_ = Rot([kb.sb([128, 64], F32) for _ in range(2)])
        otb = Rot([kb.sb([128, 4, 64], BF16) for _ in range(2)])
        ev = [0]
        ckpt(1)

        def evac(dst, src):
            ev[0] += 1
            kb.copy(dst, src, eng=("act" if ev[0] % 2 else "dve"))

        for cc in ccs:
            kb.dma(rF[:], zsT[cc * 128:(cc + 1) * 128, :])
            kb.dma(kF[:], zsT[512 + cc * 128:512 + (cc + 1) * 128, :], q="act")
            kb.dma(s1[:], zsT[1024 + cc * 128:1024 + (cc + 1) * 128, :])
            for g in range(9):
                p = prep.next()
                for j in range(4):
                    n = g * 4 + j
                    kb.tr(p[0:64, j * 128:(j + 1) * 128], s1[:, n * 64:(n + 1) * 64], C["ident"][:])
                evac(vT[:, g * 4:(g + 1) * 4, :], (p, p.t[0:64, :].rearrange("p (a b) -> p a b", a=4)))
            ckpt(2)
            kb.ts(kkF[:], kF[:], colsT[:, 16 + cc:17 + cc], None, ALU.mult)
            kb.tt(s2[:], kkF[:], kkF[:], ALU.mult)
            for (lo, n) in BLKS:
                p = prep.next()
                kb.mm(p[:, 0:n], C["blk64"][:], s2[:, lo:lo + n])
                kb.ts(s3[:, lo:lo + n], p[:, 0:n], 1e-24, None, ALU.max)
            kb.act(s3[:], s3[:], AF.Sqrt)
            kb.recip(s3[:], s3[:])
            kb.tt(kkF[:], kkF[:], s3[:], ALU.mult)
            if dbg is not None and "kk" in dbg:
                kb.dma(dbg["kk"][cc * 128:(cc + 1) * 128, :], kkF[:])
            ckpt(3)
            for d in dirs:
                dp = slice(d * 64, d * 64 + 64)
                col = d * 4 + cc
                for (lo, n) in BLKS:
                    p = prep.next()
                    kb.mm(p[:, 0:n], w2s[dp, cc * 128:(cc + 1) * 128], twd[dp, lo:lo + n])
                    kb.act(s1[:, lo:lo + n], p[:, 0:n], AF.Sigmoid, bias=colsT[:, col:col + 1])
                kb.op("dve", lambda E, o=s2.t[:], a=mseg.t[:], b=s1.t[:]: E.tensor_tensor_scan(o, a, b, 0.0, ALU.mult, ALU.add), [mseg, s1], [s2])
                if d == 0:
                    kb.tt(s1[:], s2[:], s1[:], ALU.subtract)
                    cI, cE = s2, s1
                else:
                    kb.copy(tot[:], s2[:, 63:T:64])
                    v3 = lambda b: (b, b.t[:].rearrange("p (n c) -> p n c", c=64))
                    kb.tt(v3(s2), (tot, tot.t[:].unsqueeze(2).to_broadcast([128, 36, 64])), v3(s2), ALU.subtract)
                    kb.tt(s1[:], s2[:], s1[:], ALU.add)
                    cI, cE = s1, s2
                kb.act(s3[:], cI[:], AF.Exp, scale=-CDEC)
                kb.act(cE[:], cE[:], AF.Exp, scale=-CDEC)
                kb.act(cI[:], cI[:], AF.Exp, scale=CDEC)
                eI, eE, eN = s3, cE, cI
                if d == 0:
                    kb.copy(pCt[:], eI[:, 63:T:64])
                else:
                    kb.copy(pCt[:], eI[:, 0:T:64])
                if dbg is not None and "eI" in dbg and cc == 0:
                    kb.dma(dbg["eI"][d], eI[:])
                ckpt(4)
                for (lo, n) in BLKS:
                    p = prep.next()
                    kb.mm(p[:, 0:n], a2s[dp, cc * 128:(cc + 1) * 128], adf[dp, lo:lo + n])
                    kb.act(s4[:, lo:lo + n], p[:, 0:n], AF.Sigmoid, bias=colsT[:, 8 + col:9 + col])
                kb.ts(s5[:], s4[:], -1.0, colsT[:, 20 + cc:21 + cc], ALU.add, ALU.mult)
                kb.stt(s5[:], s5[:], 1.0, kF[:], ALU.add, ALU.mult)
                if dbg is not None and "kd" in dbg and cc == 0:
                    kb.dma(dbg["kd"][d], s5[:])
                ARv = (AR, AR.t[:, :, 0:64])
                RRv = (AR, AR.t[:, :, 64:128])
                v3 = lambda b: (b, b.t[:].rearrange("p (n c) -> p n c", c=64))
                kb.stt(ARv, v3(kkF), -1.0, v3(eE), ALU.mult, ALU.mult)
                kb.tt(RRv, v3(rF), v3(eI), ALU.mult)
                kb.stt(s3[:], s5[:], colsT[:, 24 + cc:25 + cc], rF[:], ALU.mult, ALU.mult)
                kb.tt(s4[:], s4[:], eN[:], ALU.mult)
                kb.tt(s4[:], s4[:], kkF[:], ALU.mult)
                kb.tt(s5[:], s5[:], eN[:], ALU.mult)
                Bt, Kt = s4, s5
                ckpt(5)
                for g in range(9):
                    p = prep.next()
                    for j in range(4):
                        n = g * 4 + j
                        kb.mm(p[0:64, j * 2:(j + 1) * 2], s3[:, n * 64:(n + 1) * 64], hsel[:])
                    evac(coef[:, d, g * 4:(g + 1) * 4, :], (p, p.t[0:64, 0:8].rearrange("p (a b) -> p a b", a=4)))
                ckpt(6)
                kb.memset(ST[:], 0.0)
                order = list(range(36)) if d == 0 else [3, 2, 1, 0] + list(range(35, 3, -1))
                for it, n in enumerate(order):
                    cs_ = slice(n * 64, (n + 1) * 64)
                    pMb, pMk = pMbk[it % 2]
                    for hp in range(2):
                        hs = slice(hp * 64, hp * 64 + 64)
                        kb.mm(pMb[:, hp * 128:(hp + 1) * 128], Bt[hs, cs_], AR[hs, n, :])
                        kb.mm(pMk[:, hp * 128:(hp + 1) * 128], Kt[hs, cs_], AR[hs, n, :])
                        kb.mm(pNT[:, hp * 64:(hp + 1) * 64], AR[hs, n, 0:64], Bt[hs, cs_])
                    kb.tr(pTM[:, 0:128], AR[:, n, 0:64], C["ident"][:])
                    kb.tr(pTM[:, 128:256], Bt[:, cs_], C["ident"][:])
                    kb.tr(pTM[:, 256:384], Kt[:, cs_], C["ident"][:])
                    Mb = Mbr_.next(); Mk = Mkr_.next(); NTs = NTr_.next(); TM = TMr_.next()
                    f2 = lambda b: (b, b.t[:].rearrange("p a b -> p (a b)"))
                    kb.tt(f2(Mb), pMb[:], (maskM, maskM.t[:, d].rearrange("p a b -> p (a b)")), ALU.mult)
                    kb.tt(f2(Mk), pMk[:], (maskM, maskM.t[:, d].rearrange("p a b -> p (a b)")), ALU.mult)
                    kb.tt(f2(NTs), pNT[:], (maskN, maskN.t[:, d].rearrange("p a b -> p (a b)")), ALU.mult)
                    kb.copy(f2(TM), pTM[:], eng="act")
                    ckpt(7)
                    Wc = Wr_.next()
                    pW = pWr.next()
                    for hp in range(2):
                        hs = slice(hp * 64, hp * 64 + 64)
                        kb.mm(pW[:, hp * 128 + 64:hp * 128 + 128], Mk[:, hp, 0:64], vT[:, n, hs])
                    kb.copy((Wc, Wc.t[:, :, 0:64]), (TM, TM.t[:, 0, :].rearrange("p (a b) -> p a b", a=2)), eng="act")
                    kb.copy((Wc, Wc.t[:, :, 64:128]), (pW, pW.t[:].rearrange("p (a b) -> p a b", a=2)[:, :, 64:128]), eng="act")
                    Np = Npr_.next()
                    kb.copy((Np, Np.t[:, :, 0:64]), (Mb, Mb.t[:, :, 0:64]), eng="act")
                    kb.copy((Np, Np.t[:, :, 64:128]), NTs[:], eng="act")
                    for j in range(6):
                        pW = pWr.next()
                        for hp in range(2):
                            kb.mm(pW[:, hp * 128:(hp + 1) * 128], Np[:, hp, 0:64], Wc[:, hp, :])
                        Wn = Wr_.next()
                        kb.tt(f2(Wn), f2(Wc), pW[:], ALU.add)
                        Wc = Wn
                        if j < 5:
                            pN = pNr.next()
                            for hp in range(2):
                                kb.mm(pN[:, hp * 128:hp * 128 + 64], Np[:, hp, 64:128], Np[:, hp, 0:64])
                                kb.mm(pN[:, hp * 128 + 64:hp * 128 + 128], Np[:, hp, 0:64], Np[:, hp, 64:128])
                            Nn = Npr_.next()
                            kb.copy(f2(Nn), pN[:], eng="act")
                            Np = Nn
                    ckpt(8)
                    for hp in range(2):
                        hs = slice(hp * 64, hp * 64 + 64)
                        kb.mm(pRh[hs, :], Wc[:, hp, 0:64], Mb[:, hp, 64:128])
                    Rh = Rhr_.next()
                    kb.tt(Rh[:], pRh[:], AR[:, n, 64:128], ALU.add)
                    for hp in range(2):
                        hs = slice(hp * 64, hp * 64 + 64)
                        kb.mm(pPhi[hs, :], Wc[:, hp, 0:64], TM[:, 1, hs], start=True, stop=False)
                        kb.mm(pPhi[hs, :], C["ident"][0:64, 0:64], C["ident"][0:64, 0:64], start=False, stop=True)
                        kb.mm(pPsi[hs, :], TM[:, 1, hs], Wc[:, hp, 64:128], start=True, stop=False)
                        kb.mm(pPsi[hs, :], TM[:, 2, hs], vT[:, n, hs], start=False, stop=True)
                    Ph = Phr_.next(); Psn = Psr_.next()
                    kb.copy(Ph[:], pPhi[:], eng="act")
                    kb.act(Psn[:], pPsi[:], AF.Copy, scale=pCt[:, n:n + 1])
                    ckpt(9)
                    for hp in range(2):
                        hs = slice(hp * 64, hp * 64 + 64)
                        kb.mm(pY[:, hp * 64:(hp + 1) * 64], Mb[:, hp, 64:128], Wc[:, hp, 64:128], start=True, stop=False)
                        kb.mm(pY[:, hp * 64:(hp + 1) * 64], Mk[:, hp, 64:128], vT[:, n, hs], start=False, stop=False)
                        kb.mm(pY[:, hp * 64:(hp + 1) * 64], Rh[hs, :], ST[hs, :], start=False, stop=True)
                    if d == dirs[0]:
                        kb.copy(ysum[:, n, :], pY[:], eng="act")
                    else:
                        kb.tt(ysum[:, n, :], ysum[:, n, :], pY[:], ALU.add)
                    for hp in range(2):
                        hs = slice(hp * 64, hp * 64 + 64)
                        kb.mm(pS[hs, :], Ph[hs, :], ST[hs, :])
                    kb.stt(ST[:], pS[:], pCt[:, n:n + 1], Psn[:], ALU.mult, ALU.add)
                    if dbg is not None and "st" in dbg and cc == 0 and it == 3:
                        kb.dma(dbg["st"][d], ST[:])
                    if it == 0:
                        ckpt(10)
            ckpt(11)
            if dbg is not None and "ysum" in dbg:
                kb.dma(dbg["ysum"][cc], ysum[:])
            kb.dma(s3[:], zsT[1792:1920, :])
            kb.act(s3[:], s3[:], AF.Sigmoid)
            kb.tt(coef[:, 0], coef[:, 0], coef[:, 1], ALU.add)
            oa = AR
            for hf in range(2):
                n0 = hf * 18
                y4 = (ysum, ysum.t[:, n0:n0 + 18, :].rearrange("p n (h v) -> p (n h) v", h=2))
                st_ = kb.sb([64, 36, 2], F32, f"gnst{cc}_{hf}")
                kb.reduce(st_[:, :, 0], y4, ALU.add)
                kb.ts(st_[:, :, 0], st_[:, :, 0], 1.0 / 64, None, ALU.mult)
                s1v = (s1, s1.t[0:64, :].rearrange("p (a b) -> p a b", b=64))
                s2v = (s2, s2.t[0:64, :].rearrange("p (a b) -> p a b", b=64))
                kb.tt(s1v, y4, (st_, st_.t[:, :, 0:1].to_broadcast([64, 36, 64])), ALU.subtract)
                kb.tt(s2v, s1v, s1v, ALU.mult)
                kb.reduce(st_[:, :, 1], s2v, ALU.add)
                kb.ts(st_[:, :, 1], st_[:, :, 1], 1.0 / 64, 64e-5, ALU.mult, ALU.add)
                kb.act(st_[:, :, 1], st_[:, :, 1], AF.Sqrt)
                kb.recip(st_[:, :, 1], st_[:, :, 1])
                kb.tt(s1v, s1v, (st_, st_.t[:, :, 1:2].to_broadcast([64, 36, 64])), ALU.mult)
                lw3 = (lnw, lnw.t[:, cc * 128:(cc + 1) * 128].rearrange("p (h v) -> p h v", h=2).unsqueeze(1).to_broadcast([64, 18, 2, 64]))
                lb3 = (lnb, lnb.t[:, cc * 128:(cc + 1) * 128].rearrange("p (h v) -> p h v", h=2).unsqueeze(1).to_broadcast([64, 18, 2, 64]))
                s1w = (s1, s1.t[0:64, :].rearrange("p (n h v) -> p n h v", h=2, v=64))
                s2w = (s2, s2.t[0:64, :].rearrange("p (n h v) -> p n h v", h=2, v=64))
                kb.tt(s1w, s1w, lw3, ALU.mult)
                kb.tt(s1w, s1w, lb3, ALU.add)
                vT4 = (vT, vT.t[:, n0:n0 + 18, :].rearrange("p n (h v) -> p n h v", h=2))
                kb.tt(s2w, vT4, (coef, coef.t[:, 0, n0:n0 + 18, :].unsqueeze(3).to_broadcast([64, 18, 2, 64])), ALU.mult)
                kb.tt(s1w, s1w, s2w, ALU.add)
                s1c = s1.t[0:64, :].rearrange("p (n c) -> p n c", c=128)
                for g in range(9):
                    p = prep.next()
                    nn = 2
                    for j in range(nn):
                        n = n0 + g * 2 + j
                        kb.mm(p[0:64, j * 128:(j + 1) * 128], s3[:, n * 64:(n + 1) * 64], g2s[:, cc * 128:(cc + 1) * 128])
                    kb.tt((oa, oa.t[0:64, n0 + g * 2:n0 + g * 2 + 2, :]), (s1, s1c[:, g * 2:g * 2 + 2, :]),
                          (p, p.t[0:64, 0:256].rearrange("p (a b) -> p a b", a=2)), ALU.mult)
            if dbg is not None and "oa" in dbg:
                kb.dma(dbg["oa"][cc], oa[0:64, :, :])
            for g in range(9):
                p = prep.next()
                for j in range(4):
                    n = g * 4 + j
                    kb.tr(p[:, j * 64:(j + 1) * 64], oa[0:64, n, :], C["ident"][0:64, 0:64])
                ob = otb.next()
                evac((ob, ob.t[:].rearrange("p a b -> p (a b)")), p[:, 0:256])
                kb.dma(OTd[cc, :, g * 256:(g + 1) * 256], (ob, ob.t[:].rearrange("p a b -> p (a b)")))


def rms_fm(kb, C, src, nch, nw_cols, dim, dst, rstd, sq, prep):
    for (lo, n) in BLKS:
        p = prep.next()
        for c in range(nch):
            kb.tt(sq[:, lo:lo + n], src[:, c, lo:lo + n], src[:, c, lo:lo + n], ALU.mult)
            kb.mm(p[:, 0:n], C["ones"][:], sq[:, lo:lo + n], start=(c == 0), stop=(c == nch - 1))
        kb.ts(rstd[:, lo:lo + n], p[:, 0:n], 1.0 / dim, EPS, ALU.mult, ALU.add)
    kb.act(rstd[:], rstd[:], AF.Sqrt)
    kb.recip(rstd[:], rstd[:])
    for c in range(nch):
        kb.stt(dst[:, c, :], src[:, c, :], nw_cols[:, c:c + 1], rstd[:], ALU.mult, ALU.mult)


def attn_block(kb, pSr, pOr, PTr, kparts, qparts, qlo, nq, ktiles, Vfn, scale, out_fn, rc):
    nj = nq // 128
    pO = pOr.next()
    for ki, kt in enumerate(ktiles):
        pS = pSr.next()
        for pi, (kb_, qb_) in enumerate(zip(kparts, qparts)):
            kb.mm(pS[:, 0:nq], kb_[:, kt * 128:(kt + 1) * 128], qb_[:, qlo:qlo + nq], start=(pi == 0), stop=(pi == len(kparts) - 1))
        PT = PTr.next()
        kb.act(PT[:, 0:nq], pS[:, 0:nq], AF.Exp, scale=scale)
        for j in range(nj):
            kb.mm(pO[:, j * 65:(j + 1) * 65], PT[:, j * 128:(j + 1) * 128], Vfn(kt), start=(ki == 0 and j == 0), stop=(ki == len(ktiles) - 1))
    kb.recip(rc[:, 0:nj], (pO, pO.t[:, 0:nj * 65].rearrange("p (j c) -> p j c", c=65)[:, :, 64]))
    for j in range(nj):
        out_fn(j, pO[:, j * 65:j * 65 + 64], rc[:, j:j + 1])


def ph_mla(kb, C, cin, zsT, W, OTd, heads=range(8)):
    with scope(kb):
        banks = [kb.ps([128, 512], F32, f"mbank{i}") for i in range(8)]
        prep = Rot([banks[0], banks[1]])
        pSr = Rot([banks[2], banks[3], banks[4]])
        pOr = Rot([banks[5], banks[6]])
        ptr_ = banks[7]
        qn = kb.sb([128, 3, T], BF16, "qn")
        kvn = kb.sb([128, 2, T], BF16, "kvn")
        krT = kb.sb([32, T], BF16, "krT")
        zr = kb.sb([32, T], F32, "zr")
        cosF = kb.sb([32, 2048], F32, "cosF")
        sinF = kb.sb([32, 2048], F32, "sinF")
        rmT = kb.sb([32, 32], F32, "rmT")
        kb.dma(cosF[:], cin["cosF"][:])
        kb.dma(sinF[:], cin["sinF"][:])
        kb.dma(rmT[:], cin["rmT"][:])
        kb.dma(zr[:], zsT[2560:2592, :])
        tmpA = kb.sb([32, 512], F32)
        tmpB = kb.sb([32, 512], F32)

        def rope(dst_bf, src_f32):
            kb.copy(dst_bf[:, 0:LC], src_f32[:, 0:LC], eng="act")
            for b in range(4):
                lo = LC + b * 512
                p = prep.next()
                kb.mm(p[0:32, :], rmT[:], src_f32[:, lo:lo + 512])
                kb.tt(tmpA[:], src_f32[:, lo:lo + 512], cosF[:, b * 512:(b + 1) * 512], ALU.mult)
                kb.tt(tmpB[:], p[0:32, :], sinF[:, b * 512:(b + 1) * 512], ALU.mult)
                kb.tt(dst_bf[:, lo:lo + 512], tmpA[:], tmpB[:], ALU.add)

        with scope(kb):
            zq = kb.sb([128, 3, T], F32, "zq")
            zkv = kb.sb([128, 2, T], F32, "zkv")
            for c in range(3):
                kb.dma(zq[:, c, :], zsT[1920 + c * 128:1920 + (c + 1) * 128, :], q=("sp" if c % 2 else "act"))
            for c in range(2):
                kb.dma(zkv[:, c, :], zsT[2304 + c * 128:2304 + (c + 1) * 128, :], q=("sp" if c % 2 else "act"))
            nwc = kb.sb([128, 5], F32)
            colload(kb, C, nwc[:, 0:3], (W["qnw"], W["qnw"].t.ap().rearrange("(c p) -> c p", p=128)), 3, pt=banks[7])
            colload(kb, C, nwc[:, 3:5], (W["kvnw"], W["kvnw"].t.ap().rearrange("(c p) -> c p", p=128)), 2, pt=banks[7])
            rstd = kb.sb([128, T], F32)
            sq = kb.sb([128, T], F32)
            rms_fm(kb, C, zq, 3, sub(nwc, nwc.t[:, 0:3]), 384.0, qn, rstd, sq, prep)
            rms_fm(kb, C, zkv, 2, sub(nwc, nwc.t[:, 3:5]), 256.0, kvn, rstd, sq, prep)
        rope(krT, zr)
        wq = kb.sb([128, 3, 768], BF16, "wq")
        wkv = kb.sb([128, 2, 1024], BF16, "wkv")
        kb.dma(wq[:], (W["wqup"], W["wqup"].t.ap().rearrange("(c p) n -> p c n", p=128)), q="pool")
        kb.dma(wkv[:], (W["wkvup"], W["wkvup"].t.ap().rearrange("(c p) n -> p c n", p=128)), q="pool")
        Vt = kb.sb([128, NT, 8, 65], BF16, "Vt")
        kb.memset((Vt, Vt.t[:, :, :, 64:65]), 1.0)
        for i in range(NT):
            p = prep.next()
            for k in range(2):
                kb.mm(p[:], kvn[:, k, i * 128:(i + 1) * 128],
                      (wkv, wkv.t[:, k, :].rearrange("p (h x) -> p h x", x=128)[:, :, 64:128]), start=(k == 0), stop=(k == 1))
            kb.copy((Vt, Vt.t[:, i, :, 0:64]), (p, p.t[:].rearrange("p (h x) -> p h x", x=64)), eng=("act" if i % 2 else "dve"))
        qN = kb.sb([64, T], BF16, "qN")
        qr = kb.sb([32, T], F32, "qr")
        qR = kb.sb([32, T], BF16, "qR")
        kN = kb.sb([64, T], BF16, "kN")
        PTr = Rot([kb.sb([128, 512], BF16) for _ in range(3)])
        OB = kb.sb([128, NT, 128], F32, "OB")
        rc = kb.sb([128, 4], F32)
        obt = Rot([kb.sb([128, 128], BF16) for _ in range(2)])
        scale = 96.0 ** -0.5
        for h in heads:
            pr3 = Rot([banks[0], banks[1]])
            proj_fm(kb, wq, h * 96, 64, qn, 3, lambda lo, n: qN[:, lo:lo + n], pr3)
            proj_fm(kb, wq, h * 96 + 64, 32, qn, 3, lambda lo, n: qr[:, lo:lo + n], pr3)
            proj_fm(kb, wkv, h * 128, 64, kvn, 2, lambda lo, n: kN[:, lo:lo + n], pr3)
            rope(qR, qr)
            hc = (h % 2) * 64

            def out_fn_t(t0):
                def f(j, src, r):
                    kb.ts(OB[:, t0 + j, hc:hc + 64], src, r, None, ALU.mult)
                return f
            attn_block(kb, pSr, pOr, PTr, [kN, krT], [qN, qR], 0, 256, [0, 1], lambda kt: Vt[:, kt, h, :], scale, out_fn_t(0), rc)
            for b in range(4):
                attn_block(kb, pSr, pOr, PTr, [kN, krT], [qN, qR], LC + b * 512, 512, list(range(NT)), lambda kt: Vt[:, kt, h, :], scale, out_fn_t(2 + b * 4), rc)
            if h % 2 == 1:
                for i in range(NT):
                    kb.tr(ptr_[:, 0:128], OB[:, i, :], C["ident"][:])
                    ob = obt.next()
                    kb.copy(ob[:], ptr_[:, 0:128], eng=("act" if i % 2 else "dve"))
                    kb.dma(OTd[4 + h // 2, :, i * 128:(i + 1) * 128], ob[:])


def ph_outproj(kb, C, OTd, w_out, n_in, src, modv, kg, dst):
    nk = n_in // 128
    with scope(kb):
        OT = kb.sb([128, nk, T], BF16, "OT")
        for k in range(nk):
            kb.dma(OT[:, k, :], OTd[k], q=("sp" if k % 2 else "act"))
        wo = kb.sb([128, nk, 1024], BF16, "wo")
        kb.dma(wo[:], (w_out, w_out.t.ap().rearrange("(c p) n -> p c n", p=128)), q="pool")
        gb = [bload(kb, modv, 1, kg), bload(kb, modv, 0, kg)]
        xr = Rot([kb.sb([128, 1024], F32) for _ in range(2)])
        yr = Rot([kb.sb([128, 1024], F32) for _ in range(2)])
        pr = Rot([kb.ps([128, 512], F32) for _ in range(4)])
        for i in range(NT):
            w = 0 if i < 2 else 1
            xt = xr.next()
            kb.dma(xt[:], src[i * 128:(i + 1) * 128, :])
            yt = yr.next()
            for hf in range(2):
                p = pr.next()
                for k in range(nk):
                    kb.mm(p[:], OT[:, k, i * 128:(i + 1) * 128], wo[:, k, hf * 512:(hf + 1) * 512], start=(k == 0), stop=(k == nk - 1))
                kb.tt(yt[:, hf * 512:(hf + 1) * 512], p[:], gb[w][:, hf * 512:(hf + 1) * 512], ALU.mult)
            kb.tt(yt[:], yt[:], xt[:], ALU.add)
            kb.dma(dst[i * 128:(i + 1) * 128, :], yt[:], q="act")


def ph_route(kb, C, cin, xm, modv, w_router, XeT, rank_d, gm_d):
    with scope(kb):
        banks = [kb.ps([128, 512], F32, f"rbank{i}") for i in range(8)]
        h2 = kb.sb([128, NT, 1024], BF16, "h2TM")
        lgT = kb.sb([16, T], F32, "lgT")
        wr = kb.sb([128, 8, 16], F32, "wr")
        kb.dma(wr[:], (w_router, w_router.t.ap().rearrange("(c p) e -> p c e", p=128)))
        hfT = Rot([kb.sb([128, 8, 128], F32) for _ in range(2)])

        def router_tile(i, hf):
            t_ = hfT.next()
            for g in range(2):
                pb = banks[g]
                for c in range(4):
                    kb.tr(pb[:, c * 128:(c + 1) * 128], hf[:, (g * 4 + c) * 128:(g * 4 + c + 1) * 128], C["ident"][:])
                kb.copy((t_, t_.t[:, g * 4:(g + 1) * 4, :].rearrange("p a b -> p (a b)")), pb[:], eng=("act" if g else "dve"))
            pl = banks[2 + (i % 2)]
            for k in range(8):
                kb.mm(pl[0:16, 0:128], wr[:, k, :], t_[:, k, :], start=(k == 0), stop=(k == 7))
            kb.copy(lgT[:, i * 128:(i + 1) * 128], pl[0:16, 0:128], eng="act")

        with scope(kb):
            ph_norm(kb, C, xm, modv, 3, 4, hTM=h2, hTf=router_tile)
        aff = kb.sb([16, T], F32, "aff")
        kb.act(lgT[:], lgT[:], AF.Exp)
        for (lo, n) in BLKS:
            p = banks[4]
            kb.mm(p[0:16, 0:n], C["ones"][0:16, 0:16], lgT[:, lo:lo + n])
            kb.recip(aff[:, lo:lo + n], p[0:16, 0:n])
        kb.tt(aff[:], aff[:], lgT[:], ALU.mult)
        work = kb.sb([16, T], F32, "work")
        kb.copy(work[:], aff[:])
        mx = kb.sb([16, 2, 8], F32, "mx")
        for seg, (lo, hi, iters) in enumerate(((LC, T, 32), (0, LC, 4))):
            for it in range(iters):
                kb.op("dve", lambda E, o=mx.t[:, seg, :], a=work.t[:, lo:hi]: E.max(out=o, in_=a), [work], [mx])
                kb.op("dve", lambda E, o=work.t[:, lo:hi], r=mx.t[:, seg, :], a=work.t[:, lo:hi]: E.match_replace(out=o, in_to_replace=r, in_values=a, imm_value=0.0), [mx, work], [work])
        mask = kb.sb([16, T], F32, "mask")
        kb.ts(mask[:, LC:T], aff[:, LC:T], mx[:, 0, 7:8], None, ALU.is_ge)
        kb.ts(mask[:, 0:LC], aff[:, 0:LC], mx[:, 1, 7:8], None, ALU.is_ge)
        gm = kb.sb([16, T], F32, "gm")
        kb.tt(gm[:], aff[:], mask[:], ALU.mult)
        rank = kb.sb([16, T], F32, "rank")
        onesT = kb.sb([16, T], F32, "onesT")
        kb.memset(onesT[:], 1.0)
        kb.op("dve", lambda E, o=rank.t[:, LC:T], a=onesT.t[:, LC:T], b=mask.t[:, LC:T]: E.tensor_tensor_scan(o, a, b, 0.0, ALU.mult, ALU.add), [onesT, mask], [rank])
        kb.op("dve", lambda E, o=rank.t[:, 0:LC], a=onesT.t[:, 0:LC], b=mask.t[:, 0:LC]: E.tensor_tensor_scan(o, a, b, 256.0, ALU.mult, ALU.add), [onesT, mask], [rank])
        kb.tt(rank[:], rank[:], mask[:], ALU.subtract)
        kb.dma(rank_d[:], rank[:])
        kb.dma(gm_d[:], gm[:])
        rmT = kb.sb([128, NT, 2, 16], F32, "rmT")
        for i in range(NT):
            p = banks[5 + (i % 2)]
            kb.tr(p[:, 0:16], rank[:, i * 128:(i + 1) * 128], C["ident"][0:16, 0:16])
            kb.tr(p[:, 16:32], mask[:, i * 128:(i + 1) * 128], C["ident"][0:16, 0:16])
            kb.copy((rmT, rmT.t[:, i].rearrange("p a b -> p (a b)")), p[:, 0:32], eng=("act" if i % 2 else "dve"))
        iota = kb.sb([128, 288], F32, "iota")
        kb.dma(iota[:], cin["iota"][:])
        Per = Rot([kb.sb([128, NT, 256], BF16) for _ in range(2)])
        xer = Rot([kb.sb([128, 8, 288], BF16) for _ in range(2)])
        pgr = Rot([banks[0], banks[1], banks[2], banks[3]])
        for e in range(16):
            Pe = Per.next()
            for i in range(NT):
                if i < 2:
                    kb.ts(Pe[:, i, 0:32], iota[:, 256:288], rmT[:, i, 0, e:e + 1], rmT[:, i, 1, e:e + 1], ALU.is_equal, ALU.mult)
                else:
                    kb.ts(Pe[:, i, :], iota[:, 0:256], rmT[:, i, 0, e:e + 1], rmT[:, i, 1, e:e + 1], ALU.is_equal, ALU.mult)
            xe = xer.next()
            for c in range(8):
                pg = pgr.next()
                for i in range(2, NT):
                    kb.mm(pg[:, 0:256], h2[:, i, c * 128:(c + 1) * 128], Pe[:, i, :], start=(i == 2), stop=(i == NT - 1))
                for i in range(2):
                    kb.mm(pg[:, 256:288], h2[:, i, c * 128:(c + 1) * 128], Pe[:, i, 0:32], start=(i == 0), stop=(i == 1))
                kb.copy(xe[:, c, :], pg[:, 0:288], eng=("act" if c % 2 else "dve"))
            kb.dma((XeT, XeT.t[e].rearrange("k p j -> p k j")), xe[:], q=("sp" if e % 2 else "act"))


def ph_ffn(kb, C, XeB, w1, w3, w2, YB, nexp=2, nsamp=8):
    with scope(kb):
        banks = [kb.ps([128, 512], F32, f"fbank{i}") for i in range(8)]
        par = Rot([banks[0], banks[1]])
        pbr = Rot([banks[2], banks[3]])
        pyr = Rot([banks[4], banks[5], banks[6], banks[7]])
        w1s = kb.sb([128, 8, 2048], BF16, "w1s")
        w3s = kb.sb([128, 8, 2048], BF16, "w3s")
        w2s = kb.sb([128, 16, 1024], BF16, "w2s")
        Xr = Rot([kb.sb([128, 8, 576], BF16) for _ in range(2)])
        hid = kb.sb([128, 16, 576], BF16, "hid")
        sar = Rot([kb.sb([128, 288], F32) for _ in range(2)])
        ysr = Rot([kb.sb([128, 1024], BF16) for _ in range(2)])
        for e in range(nexp):
            for k in range(8):
                kb.dma(w1s[:, k, :], w1[e, k * 128:(k + 1) * 128, :], q="pool")
                kb.dma(w3s[:, k, :], w3[e, k * 128:(k + 1) * 128, :], q="pool")
            for f in range(16):
                kb.dma(w2s[:, f, :], w2[e, f * 128:(f + 1) * 128, :], q="pool")
            gs = min(2, nsamp)
            for sg in range(nsamp // gs):
                X = Xr.next()
                for s in range(gs):
                    kb.dma(X[:, :, s * 288:(s + 1) * 288], (XeB, XeB.t[e, sg * gs + s].rearrange("k p j -> p k j")), q=("sp" if s else "act"))
                for f in range(16):
                    for s in range(gs):
                        pa = par.next(); pb = pbr.next()
                        for k in range(8):
                            kb.mm(pa[:, 0:288], w1s[:, k, f * 128:(f + 1) * 128], X[:, k, s * 288:(s + 1) * 288], start=(k == 0), stop=(k == 7))
                        for k in range(8):
                            kb.mm(pb[:, 0:288], w3s[:, k, f * 128:(f + 1) * 128], X[:, k, s * 288:(s + 1) * 288], start=(k == 0), stop=(k == 7))
                        sa = sar.next()
                        kb.act(sa[:], pa[:, 0:288], AF.Silu)
                        kb.tt(hid[:, f, s * 288:(s + 1) * 288], sa[:], pb[:, 0:288], ALU.mult)
                for s in range(gs):
                    for (lo, M) in ((0, 128), (128, 128), (256, 32)):
                        ys = ysr.next()
                        for hf in range(2):
                            py = pyr.next()
                            for f in range(16):
                                kb.mm(py[0:M, :], hid[:, f, s * 288 + lo:s * 288 + lo + M], w2s[:, f, hf * 512:(hf + 1) * 512], start=(f == 0), stop=(f == 15))
                            kb.copy(ys[0:M, hf * 512:(hf + 1) * 512], py[0:M, :], eng=("act" if hf else "dve"))
                        kb.dma(YB[e, sg * gs + s, lo:lo + M, :], ys[0:M, :], q=("sp" if s else "act"))


def ph_scatter(kb, C, cin, xm, Yd, rank_d, gm_d, modv_prev, dst):
    with scope(kb):
        banks = [kb.ps([128, 512], F32, f"sbank{i}") for i in range(8)]
        Ys = kb.sb([128, 16, 2, 1024], BF16, "Ys")
        Yc = kb.sb([32, 16, 1024], BF16, "Yc")
        for e in range(16):
            kb.dma(Ys[:, e, :, :], (Yd, Yd.t[e, 0:256, :].rearrange("(j p) d -> p j d", p=128)), q=("sp" if e % 2 else "act"))
        kb.dma(Yc[:], (Yd, Yd.t[:, 256:288, :].rearrange("e p d -> p e d")))
        rank = kb.sb([16, T], F32, "srank")
        gm = kb.sb([16, T], F32, "sgm")
        kb.dma(rank[:], rank_d[:])
        kb.dma(gm[:], gm_d[:])
        sel = kb.sb([16, 16, 128], F32, "sel")
        kb.dma(sel[:], cin["sel"][:])
        icol = kb.sb([128, 3], F32, "icol")
        kb.dma(icol[:], cin["icol"][:])
        gb = [bload(kb, modv_prev, 1, 5), bload(kb, modv_prev, 0, 5)]
        PTr = Rot([kb.sb([128, 16, 2, 128], BF16) for _ in range(2)])
        eqr = Rot([kb.sb([128, 128], F32) for _ in range(3)])
        xr = Rot([kb.sb([128, 1024], F32) for _ in range(2)])
        yr = Rot([kb.sb([128, 1024], F32) for _ in range(2)])
        pbr = Rot([banks[0], banks[1], banks[2], banks[3]])
        por = Rot([banks[4], banks[5], banks[6], banks[7]])
        for i in range(NT):
            ctx_t = i < 2
            PT = PTr.next()
            for e in range(16):
                pb = pbr.next()
                kb.mm(pb[:, 0:128], sel[:, e, :], rank[:, i * 128:(i + 1) * 128])
                kb.mm(pb[:, 128:256], sel[:, e, :], gm[:, i * 128:(i + 1) * 128])
                if ctx_t:
                    eq = eqr.next()
                    kb.ts(eq[0:32, :], pb[0:32, 0:128], icol[0:32, 2:3], None, ALU.is_equal)
                    kb.tt(PT[0:32, e, 0, :], eq[0:32, :], pb[0:32, 128:256], ALU.mult)
                else:
                    for jt in range(2):
                        eq = eqr.next()
                        kb.ts(eq[:], pb[:, 0:128], icol[:, jt:jt + 1], None, ALU.is_equal)
                        kb.tt(PT[:, e, jt, :], eq[:], pb[:, 128:256], ALU.mult)
            xt = xr.next()
            kb.dma(xt[:], xm[i * 128:(i + 1) * 128, :])
            yt = yr.next()
            w = 0 if ctx_t else 1
            for hf in range(2):
                po = por.next()
                if ctx_t:
                    for e in range(16):
                        kb.mm(po[:], PT[0:32, e, 0, :], Yc[:, e, hf * 512:(hf + 1) * 512], start=(e == 0), stop=(e == 15))
                else:
                    for e in range(16):
                        for jt in range(2):
                            kb.mm(po[:], PT[:, e, jt, :], Ys[:, e, jt, hf * 512:(hf + 1) * 512], start=(e == 0 and jt == 0), stop=(e == 15 and jt == 1))
                kb.tt(yt[:, hf * 512:(hf + 1) * 512], po[:], gb[w][:, hf * 512:(hf + 1) * 512], ALU.mult)
            kb.tt(yt[:], yt[:], xt[:], ALU.add)
            kb.dma(dst[i * 128:(i + 1) * 128, :], yt[:], q="act")


def ph_na(kb, C, cin, src, modv, w_qkv, bc_d, OTd, pairs=range(8)):
    with scope(kb):
        hT = kb.sb([128, 8, T], BF16, "hTn")
        with scope(kb):
            ph_norm(kb, C, src, modv, 0, 1, hT=hT)
        banks = [kb.ps([128, 512], F32, f"nbank{i}") for i in range(8)]
        prep = Rot([banks[0], banks[1]])
        pSr = Rot([banks[2], banks[3]])
        pCr = Rot([banks[4], banks[5]])
        pOr = Rot([banks[6], banks[7]])
        wq = kb.sb([128, 8, 3072], BF16, "wqkv")
        wv = w_qkv.t.ap().rearrange("(c p) n -> p c n", p=128)
        for k in range(8):
            kb.dma(wq[:, k, :], (w_qkv, wv[:, k, :]), q="pool")
        qT = kb.sb([128, T], BF16, "qTn")
        kT = kb.sb([128, T], BF16, "kTn")
        Vr = kb.sb([64, 32, 2, 65], BF16, "Vr")
        Vc = kb.sb([128, 2, 2, 65], BF16, "Vc")
        kb.memset((Vr, Vr.t[:, :, :, 64:65]), 1.0)
        kb.memset((Vc, Vc.t[:, :, :, 64:65]), 1.0)
        bc = kb.sb([64, 2, 15, 64], F32, "bc")
        sbr = Rot([kb.sb([64, 512], F32) for _ in range(2)])
        Pr = Rot([kb.sb([64, 512], BF16) for _ in range(2)])
        Pcr = Rot([kb.sb([128, 128], BF16) for _ in range(2)])
        PTr = Rot([kb.sb([128, 512], BF16) for _ in range(2)])
        OBr = kb.sb([64, 32, 128], F32, "OBr")
        OBc = kb.sb([128, 2, 128], F32, "OBc")
        rc = kb.sb([128, 4], F32)
        obt = Rot([kb.sb([128, 256], BF16) for _ in range(2)])
        scale = 64.0 ** -0.5
        for pr_ in pairs:
            proj_fm(kb, wq, pr_ * 128, 128, hT, 8, lambda lo, n: qT[:, lo:lo + n], prep)
            proj_fm(kb, wq, 1024 + pr_ * 128, 128, hT, 8, lambda lo, n: kT[:, lo:lo + n], prep)
            kb.dma(bc[:], bc_d[:, pr_ * 2:pr_ * 2 + 2, :, :])
            for g in range(8):
                p = prep.next()
                for j in range(4):
                    row = g * 4 + j
                    for k in range(8):
                        kb.mm(p[0:64, j * 128:(j + 1) * 128], hT[:, k, LC + row * 64:LC + (row + 1) * 64], wq[:, k, 2048 + pr_ * 128:2048 + (pr_ + 1) * 128], start=(k == 0), stop=(k == 7))
                kb.copy((Vr, Vr.t[:, g * 4:(g + 1) * 4, :, 0:64]), (p, p.t[0:64, :].rearrange("p (r h v) -> p r h v", r=4, h=2)), eng=("act" if g % 2 else "dve"))
            p = prep.next()
            for ct in range(2):
                for k in range(8):
                    kb.mm(p[:, ct * 128:(ct + 1) * 128], hT[:, k, ct * 128:(ct + 1) * 128], wq[:, k, 2048 + pr_ * 128:2048 + (pr_ + 1) * 128], start=(k == 0), stop=(k == 7))
            kb.copy((Vc, Vc.t[:, :, :, 0:64]), (p, p.t[:, 0:256].rearrange("p (r h v) -> p r h v", r=2, h=2)))
            for hh in range(2):
                hs = slice(hh * 64, hh * 64 + 64)
                hc = hh * 64
                def ofn(j, src_, r):
                    kb.ts(OBc[:, j, hc:hc + 64], src_, r, None, ALU.mult)
                attn_block(kb, pSr, pOr, PTr, [sub(kT, kT.t[hs, :])], [sub(qT, qT.t[hs, :])], 0, 256, [0, 1], lambda kt: Vc[:, kt, hh, :], scale, ofn, rc)
                for qr in range(32):
                    rs = min(max(qr - 4, 0), 24)
                    dr0 = rs - qr + 7
                    q0 = LC + qr * 64
                    pS = pSr.next()
                    for j in range(8):
                        k0 = LC + (rs + j) * 64
                        kb.mm(pS[0:64, j * 64:(j + 1) * 64], kT[hs, k0:k0 + 64], qT[hs, q0:q0 + 64])
                    pC = pCr.next()
                    for ct in range(2):
                        kb.mm(pC[:, ct * 64:(ct + 1) * 64], kT[hs, ct * 128:(ct + 1) * 128], qT[hs, q0:q0 + 64])
                    sb_ = sbr.next()
                    kb.stt(sb_[:], pS[0:64, :], scale, (bc, bc.t[:, hh, dr0:dr0 + 8, :].rearrange("p a b -> p (a b)")), ALU.mult, ALU.add)
                    P = Pr.next()
                    kb.act(P[:], sb_[:], AF.Exp)
                    Pc = Pcr.next()
                    kb.act(Pc[:], pC[:, 0:128], AF.Exp, scale=scale)
                    pO = pOr.next()
                    for j in range(8):
                        kb.mm(pO[0:64, 0:65], P[:, j * 64:(j + 1) * 64], Vr[:, rs + j, hh, :], start=(j == 0), stop=False)
                    for ct in range(2):
                        kb.mm(pO[0:64, 0:65], Pc[:, ct * 64:(ct + 1) * 64], Vc[:, ct, hh, :], start=False, stop=(ct == 1))
                    kb.recip(rc[0:64, 0:1], pO[0:64, 64:65])
                    kb.ts(OBr[:, qr, hc:hc + 64], pO[0:64, 0:64], rc[0:64, 0:1], None, ALU.mult)
            for g in range(8):
                p = prep.next()
                for j in range(4):
                    kb.tr(p[:, j * 64:(j + 1) * 64], OBr[:, g * 4 + j, :], C["ident"][0:64, 0:64])
                ob = obt.next()
                kb.copy(ob[:], p[:, 0:256], eng=("act" if g % 2 else "dve"))
                kb.dma(OTd[pr_, :, LC + g * 256:LC + (g + 1) * 256], ob[:])
            p = prep.next()
            for ct in range(2):
                kb.tr(p[:, ct * 128:(ct + 1) * 128], OBc[:, ct, :], C["ident"][:])
            ob = obt.next()
            kb.copy(ob[:], p[:, 0:256])
            kb.dma(OTd[pr_, :, 0:256], ob[:])


import ml_dtypes

NCORES = 8


def _consts():
    c = {}
    c["ident"] = np.eye(128, dtype=np.float32)
    c["blk64"] = np.kron(np.eye(2), np.ones((64, 64))).astype(np.float32)
    m = np.ones((128, T), np.float32)
    m[:, 0::64] = 0
    c["mseg"] = m
    su = np.triu(np.ones((64, 64), np.float32), 1)
    ui = np.triu(np.ones((64, 64), np.float32), 0)
    mm = np.zeros((64, 2, 2, 128), np.float32)
    mm[:, 0, :, 0:64] = su[:, None, :]
    mm[:, 0, :, 64:] = ui[:, None, :]
    mm[:, 1, :, 0:64] = su.T[:, None, :]
    mm[:, 1, :, 64:] = ui.T[:, None, :]
    c["maskM"] = mm
    mn = np.zeros((64, 2, 2, 64), np.float32)
    mn[:, 0] = su.T[:, None, :]
    mn[:, 1] = su[:, None, :]
    c["maskN"] = mn
    hs = np.zeros((128, 2), np.float32)
    hs[:64, 0] = 1
    hs[64:, 1] = 1
    c["hsel"] = hs
    t = np.arange(2048)
    row = (t // 64).astype(np.float32)
    col = (t % 64).astype(np.float32)
    inv = (np.float32(10000.0) ** (-np.arange(8, dtype=np.float32) / np.float32(8))).astype(np.float32)
    ang = np.concatenate([row[:, None] * inv, col[:, None] * inv], -1).astype(np.float32)
    c["cosF"] = np.ascontiguousarray(np.repeat(np.cos(ang).astype(np.float32).T, 2, axis=0))
    c["sinF"] = np.ascontiguousarray(np.repeat(np.sin(ang).astype(np.float32).T, 2, axis=0))
    Rm = np.zeros((32, 32), np.float32)
    for i in range(16):
        Rm[2 * i, 2 * i + 1] = -1
        Rm[2 * i + 1, 2 * i] = 1
    c["rmT"] = np.ascontiguousarray(Rm.T)
    c["iota"] = np.tile(np.arange(288, dtype=np.float32), (128, 1))
    sel = np.zeros((16, 16, 128), np.float32)
    for e in range(16):
        sel[e, e, :] = 1
    c["sel"] = sel
    c["icol"] = np.stack([np.arange(128), np.arange(128) + 128, np.arange(128) + 256], 1).astype(np.float32)
    return c


def _make_bc(rpb):
    qc = np.arange(64)
    kc = np.arange(64)
    cs = np.clip(qc - 8, 0, 48)
    valid = (kc[:, None] >= cs[None, :]) & (kc[:, None] < cs[None, :] + 16)
    dc = np.clip(kc[:, None] - qc[None, :] + 15, 0, 30)
    g = rpb[:, :, dc]
    g = np.where(valid[None, None], g, np.float32(-1e30)).astype(np.float32)
    return np.ascontiguousarray(g.transpose(2, 0, 1, 3))


EVEN_W = {"w_in": [1024, 2592], "mu": [2, 1920], "w0": [2, 512], "w2": [2, 64, 512], "a0": [2, 512], "a2": [2, 64, 512],
          "g2": [128, 512], "kk": [512], "ka": [512], "rk": [8, 64], "lnw": [512], "lnb": [512],
          "qnw": [384], "wqup": [384, 768], "kvnw": [256], "wkvup": [256, 1024]}
ODD_W = {"w_qkv": [1024, 3072], "bc": [64, 16, 15, 64]}
CONST_SHAPES = {k: list(v.shape) for k, v in _consts().items()}


def _mk(nc):
    def din(name, shape, dt=F32):
        return Buf(nc.dram_tensor(name, list(shape), dt, kind="ExternalInput"), name)

    def dout(name, shape, dt=F32):
        return Buf(nc.dram_tensor(name, list(shape), dt, kind="ExternalOutput"), name)
    return din, dout


def build_A(even, with_scatter):
    nc = bass.Bass("TRN2", target_bir_lowering=False)
    din, dout = _mk(nc)
    xin = din("xin", [T, 1024])
    c2 = din("c2", [16, 128])
    mod_w = din("mod_w", [1024, 6144])
    mod_b = din("mod_b", [6144])
    n1w = din("n1w", [1024])
    n2w = din("n2w", [1024])
    w_router = din("w_router", [1024, 16])
    w_out = din("w_out", [1024, 1024])
    cin = {k: din(k, v) for k, v in CONST_SHAPES.items()}
    W = {k: din(k, v) for k, v in (EVEN_W if even else ODD_W).items()}
    if with_scatter:
        Yd = din("Yd", [16, 288, 1024], BF16)
        rank_p = din("rank_p", [16, T])
        gm_p = din("gm_p", [16, T])
        modv_p = din("modv_p", [2, 6144])
    xm = dout("xm", [T, 1024])
    XeT = dout("XeT", [16, 8, 128, 288], BF16)
    rank_d = dout("rank", [16, T])
    gm_d = dout("gm", [16, T])
    modv = dout("modv", [2, 6144])
    with ExitStack() as st:
        kb = KB(nc, st)
        OTd = kb.dram("OTd", [8, 128, T], BF16)
        C = load_consts(kb, cin)
        src = xin
        if with_scatter:
            xcur = kb.dram("xcur", [T, 1024])
            ph_scatter(kb, C, cin, xin, Yd, rank_p, gm_p, modv_p, xcur)
            src = xcur
        ph_mod(kb, C, c2, mod_w, mod_b, n1w, n2w, modv)
        if even:
            zsT = kb.dram("zsT", [2592, T])
            ph_inproj(kb, C, src, modv, W["w_in"], W["mu"], zsT)
            ph_rwkv(kb, C, cin, zsT, W, OTd)
            ph_mla(kb, C, cin, zsT, W, OTd)
        else:
            ph_na(kb, C, cin, src, modv, W["w_qkv"], W["bc"], OTd)
        ph_outproj(kb, C, OTd, w_out, 1024, src, modv, 2, xm)
        ph_route(kb, C, cin, xm, modv, w_router, XeT, rank_d, gm_d)
        kb.emit_block(final=True)
    return nc


def build_B():
    nc = bass.Bass("TRN2", target_bir_lowering=False)
    din, dout = _mk(nc)
    XeB = din("XeB", [2, 8, 8, 128, 288], BF16)
    w1 = din("w1", [2, 1024, 2048])
    w3 = din("w3", [2, 1024, 2048])
    w2 = din("w2", [2, 2048, 1024])
    YB = dout("YB", [2, 8, 288, 1024], BF16)
    with ExitStack() as st:
        kb = KB(nc, st)
        ph_ffn(kb, None, XeB, w1, w3, w2, YB, nexp=2, nsamp=8)
        kb.emit_block(final=True)
    return nc


def build_F():
    nc = bass.Bass("TRN2", target_bir_lowering=False)
    din, dout = _mk(nc)
    xin = din("xin", [T, 1024])
    Yd = din("Yd", [16, 288, 1024], BF16)
    rank_p = din("rank_p", [16, T])
    gm_p = din("gm_p", [16, T])
    modv_p = din("modv_p", [2, 6144])
    fnw = din("fnw", [1024])
    cin = {k: din(k, CONST_SHAPES[k]) for k in ("ident", "blk64", "sel", "icol")}
    out = dout("out", [2048, 1024])
    with ExitStack() as st:
        kb = KB(nc, st)
        C = load_consts(kb, cin)
        xcur = kb.dram("xcur", [T, 1024])
        ph_scatter(kb, C, cin, xin, Yd, rank_p, gm_p, modv_p, xcur)
        with scope(kb):
            wb = kb.sb([128, 1024], F32)
            kb.dma(wb[:], (fnw, fnw.t.ap().partition_broadcast(128)))
            xr = Rot([kb.sb([128, 1024], F32) for _ in range(2)])
            yr = Rot([kb.sb([128, 1024], F32) for _ in range(2)])
            junk = kb.sb([128, 1024], F32)
            sr = Rot([kb.sb([128, 2], F32) for _ in range(2)])
            for i in range(2, NT):
                xt = xr.next()
                kb.dma(xt[:], xcur[i * 128:(i + 1) * 128, :])
                s = sr.next()
                kb.memset(s[:], 0.0)
                kb.act(junk[:], xt[:], AF.Square, accum=s[:, 0:1])
                kb.ts(s[:, 1:2], s[:, 0:1], 1.0 / 1024, EPS, ALU.mult, ALU.add)
                kb.act(s[:, 1:2], s[:, 1:2], AF.Sqrt)
                kb.recip(s[:, 1:2], s[:, 1:2])
                yt = yr.next()
                kb.stt(yt[:], xt[:], s[:, 1:2], wb[:], ALU.mult, ALU.mult)
                kb.dma(out[(i - 2) * 128:(i - 1) * 128, :], yt[:], q="act")
        kb.emit_block(final=True)
    return nc


_PROGS = {}


def _prog(key, fn):
    if key not in _PROGS:
        _PROGS[key] = fn()
    return _PROGS[key]


def _run(nc, in_maps):
    import time
    t0 = time.time()
    res = run_bass_kernel_spmd(nc, in_maps, core_ids=list(range(NCORES)))
    if os.environ.get("KDEBUG"):
        print("[kernel] launch took %.1fs" % (time.time() - t0), flush=True)
    return res.results


def kernel(x, c, ctx, c_ctx, mod_w, mod_b, norm1_w, norm2_w, final_norm_w, ab_w_in, ab_w_out,
           rk_mu, rk_w0, rk_w2, rk_a0, rk_a2, rk_g2, rk_kk, rk_ka, rk_rk, rk_ln_w, rk_ln_b,
           mla_qn_w, mla_w_qup, mla_kvn_w, mla_w_kvup, na_w_qkv, na_rpb, na_w_out,
           moe_router, moe_w1, moe_w3, moe_w2):
    f32 = lambda a: np.ascontiguousarray(np.asarray(a, dtype=np.float32))
    x, c, ctx, c_ctx = f32(x), f32(c), f32(ctx), f32(c_ctx)
    CN = _consts()
    xin = [np.concatenate([ctx[b], x[b]], 0) for b in range(NCORES)]
    c2 = [np.concatenate([c[b].reshape(8, 128), c_ctx.reshape(8, 128)], 0) for b in range(NCORES)]
    prev = None
    for l in range(4):
        even = (l % 2 == 0)
        i = l // 2
        nc = _prog(("A", even, l > 0), lambda: build_A(even, l > 0))
        shared = {"mod_w": f32(mod_w[l]), "mod_b": f32(mod_b[l]), "n1w": f32(norm1_w[l]), "n2w": f32(norm2_w[l]),
                  "w_router": f32(moe_router[l])}
        shared.update(CN)
        if even:
            shared.update({"w_in": f32(ab_w_in[i]), "mu": f32(rk_mu[i]), "w0": f32(rk_w0[i]), "w2": f32(rk_w2[i]), "a0": f32(rk_a0[i]),
                           "a2": f32(rk_a2[i]), "g2": f32(rk_g2[i]), "kk": f32(rk_kk[i]), "ka": f32(rk_ka[i]), "rk": f32(rk_rk[i]),
                           "lnw": f32(rk_ln_w[i]), "lnb": f32(rk_ln_b[i]), "qnw": f32(mla_qn_w[i]), "wqup": f32(mla_w_qup[i]),
                           "kvnw": f32(mla_kvn_w[i]), "wkvup": f32(mla_w_kvup[i]), "w_out": f32(ab_w_out[i])})
        else:
            shared.update({"w_qkv": f32(na_w_qkv[i]), "bc": _make_bc(f32(na_rpb[i])), "w_out": f32(na_w_out[i])})
        maps = []
        for b in range(NCORES):
            m = dict(shared)
            m["c2"] = c2[b]
            if l == 0:
                m["xin"] = xin[b]
            else:
                m["xin"] = prev[b]["xm"]
                m["Yd"] = prev[b]["Yd"]
                m["rank_p"] = prev[b]["rank"]
                m["gm_p"] = prev[b]["gm"]
                m["modv_p"] = prev[b]["modv"]
            maps.append(m)
        outA = _run(nc, maps)
        ncB = _prog(("B",), build_B)
        mapsB = []
        for r in range(NCORES):
            XeB = np.ascontiguousarray(np.stack([np.asarray(outA[b]["XeT"])[2 * r:2 * r + 2] for b in range(NCORES)], axis=1))
            mapsB.append({"XeB": XeB, "w1": f32(moe_w1[l, 2 * r:2 * r + 2]), "w3": f32(moe_w3[l, 2 * r:2 * r + 2]),
                          "w2": f32(moe_w2[l, 2 * r:2 * r + 2])})
        outB = _run(ncB, mapsB)
        prev = []
        for b in range(NCORES):
            Yd = np.ascontiguousarray(np.concatenate([np.asarray(outB[r]["YB"])[:, b] for r in range(NCORES)], axis=0))
            prev.append({"xm": np.asarray(outA[b]["xm"]), "rank": np.asarray(outA[b]["rank"]), "gm": np.asarray(outA[b]["gm"]),
                         "modv": np.asarray(outA[b]["modv"]), "Yd": Yd})
    ncF = _prog(("F",), build_F)
    mapsF = []
    for b in range(NCORES):
        m = {k: CN[k] for k in ("ident", "blk64", "sel", "icol")}
        m.update({"xin": prev[b]["xm"], "Yd": prev[b]["Yd"], "rank_p": prev[b]["rank"], "gm_p": prev[b]["gm"],
                  "modv_p": prev[b]["modv"], "fnw": f32(final_norm_w)})
        mapsF.append(m)
    outF = _run(ncF, mapsF)
    return np.stack([np.asarray(outF[b]["out"], dtype=np.float32) for b in range(NCORES)], axis=0)
```
